# Optimizing a Trainium2 kernel written in Bass

```python
import math
import jax, jax.numpy as jnp
from jax import lax
import numpy as np

D_MODEL = 1024
BATCH = 8
SEQ = 2048
DEPTH = 1

D_MIX = D_MODEL
GLA_WIDTH = D_MIX // 2
S5_WIDTH = D_MIX - GLA_WIDTH
GLA_HEADS = 4
GLA_DV = GLA_WIDTH // GLA_HEADS
GLA_DK = GLA_DV // 2
GLA_LOWRANK = 16
GLA_TAU = 16.0
GLA_CHUNK = 64
S5_CH = 16
S5_GROUPS = S5_WIDTH // S5_CH
S5_STATE = 64
DT_MIN = 1e-3
DT_MAX = 1e-1
N_EXPERTS = 32
TOP_K = 4
D_FF = D_MODEL
SWIGLU_ALPHA = 1.702
SWIGLU_LIMIT = 7.0
MOE_BLOCK = 128
EPS = 1e-6
Q_COLS = GLA_HEADS * GLA_DK
K_COLS = GLA_HEADS * GLA_DK
V_COLS = GLA_WIDTH
G_COLS = GLA_WIDTH
A_COLS = GLA_LOWRANK
U_COLS = S5_WIDTH
N_IN = Q_COLS + K_COLS + V_COLS + G_COLS + A_COLS + U_COLS
SPLITS = [Q_COLS, Q_COLS + K_COLS, Q_COLS + K_COLS + V_COLS, Q_COLS + K_COLS + V_COLS + G_COLS,
          Q_COLS + K_COLS + V_COLS + G_COLS + A_COLS]

kernel_name = "hymba_gla_s5_moe_block"


def rmsnorm(x, g):
    xf = x.astype(jnp.float32)
    r = lax.rsqrt(jnp.mean(xf * xf, axis=-1, keepdims=True) + EPS)
    return (xf * r * g.astype(jnp.float32)).astype(x.dtype)


def gla_chunked(q, k, v, log_a):
    f32 = jnp.float32
    B_, H_, T_, DK_ = q.shape
    DV_ = v.shape[-1]
    C = GLA_CHUNK
    nc = T_ // C
    q = q.astype(f32).reshape(B_, H_, nc, C, DK_) * (DK_ ** -0.5)
    k = k.astype(f32).reshape(B_, H_, nc, C, DK_)
    v = v.astype(f32).reshape(B_, H_, nc, C, DV_)
    b = jnp.cumsum(log_a.astype(f32).reshape(B_, H_, nc, C, DK_), axis=3)
    b_last = b[:, :, :, -1:, :]
    q_dec = q * jnp.exp(b)
    k_inv = k * jnp.exp(-b)
    k_to_end = k * jnp.exp(b_last - b)
    causal = jnp.tril(jnp.ones((C, C), dtype=bool))
    scores = jnp.where(causal, jnp.einsum('bhnik,bhnjk->bhnij', q_dec, k_inv), 0.0)
    o_intra = jnp.einsum('bhnij,bhnjv->bhniv', scores, v)
    delta = jnp.einsum('bhnjk,bhnjv->bhnkv', k_to_end, v)
    decay = jnp.exp(b_last[:, :, :, 0, :])

    def step(S, inp):
        d, dS = inp
        return d[..., None] * S + dS, S

    S0 = jnp.zeros((B_, H_, DK_, DV_), f32)
    _, S_prev = lax.scan(step, S0, (jnp.moveaxis(decay, 2, 0), jnp.moveaxis(delta, 2, 0)))
    S_prev = jnp.moveaxis(S_prev, 0, 2)
    o_inter = jnp.einsum('bhnik,bhnkv->bhniv', q_dec, S_prev)
    return (o_intra + o_inter).reshape(B_, H_, T_, DV_)


def s5_layer(u, lam_re, lam_im, log_dt, b_re, b_im, c_re, c_im, d, glu_w, glu_b):
    f32 = jnp.float32
    B_, T_, _ = u.shape
    uf = u.astype(f32).reshape(B_, T_, S5_GROUPS, S5_CH)
    lr = lam_re.astype(f32)
    li = lam_im.astype(f32)
    dt = jnp.exp(log_dt.astype(f32))[:, None]
    mag = jnp.exp(lr * dt)
    ab_re = mag * jnp.cos(li * dt)
    ab_im = mag * jnp.sin(li * dt)
    den = lr * lr + li * li
    f_re = ((ab_re - 1.0) * lr + ab_im * li) / den
    f_im = (ab_im * lr - (ab_re - 1.0) * li) / den
    br = b_re.astype(f32)
    bi = b_im.astype(f32)
    bb_re = f_re[..., None] * br - f_im[..., None] * bi
    bb_im = f_re[..., None] * bi + f_im[..., None] * br
    bu_re = jnp.einsum('btgh,gph->btgp', uf, bb_re)
    bu_im = jnp.einsum('btgh,gph->btgp', uf, bb_im)
    a_re = jnp.broadcast_to(ab_re, (1, T_, S5_GROUPS, S5_STATE))
    a_im = jnp.broadcast_to(ab_im, (1, T_, S5_GROUPS, S5_STATE))

    def combine(e1, e2):
        a1r, a1i, b1r, b1i = e1
        a2r, a2i, b2r, b2i = e2
        return (a2r * a1r - a2i * a1i,
                a2r * a1i + a2i * a1r,
                a2r * b1r - a2i * b1i + b2r,
                a2r * b1i + a2i * b1r + b2i)

    _, _, xr, xi = lax.associative_scan(combine, (a_re, a_im, bu_re, bu_im), axis=1)
    y = (jnp.einsum('btgp,ghp->btgh', xr, c_re.astype(f32))
         - jnp.einsum('btgp,ghp->btgh', xi, c_im.astype(f32))
         + d.astype(f32) * uf)
    y = jax.nn.gelu(y.reshape(B_, T_, S5_WIDTH))
    return y * jax.nn.sigmoid(y @ glu_w.astype(f32) + glu_b.astype(f32))


def moe_ffn(h, router_w, router_b, w_gu, b_gu, w_down, b_down):
    f32 = jnp.float32
    B_, T_, D_ = h.shape
    N = B_ * T_
    NK = N * TOP_K
    xt = h.reshape(N, D_)
    logits = (xt @ router_w + router_b).astype(f32)
    top_val, top_idx = lax.top_k(logits, TOP_K)
    gate = jax.nn.softmax(top_val, axis=-1)
    flat_e = top_idx.reshape(NK).astype(jnp.int32)
    flat_tok = jnp.arange(NK, dtype=jnp.int32) // TOP_K
    flat_w = gate.reshape(NK)
    order = jnp.argsort(flat_e)
    se, stok, sw = flat_e[order], flat_tok[order], flat_w[order]
    counts = jnp.zeros((N_EXPERTS,), jnp.int32).at[flat_e].add(1)
    starts = jnp.cumsum(counts) - counts
    padded = (counts + MOE_BLOCK - 1) // MOE_BLOCK * MOE_BLOCK
    pends = jnp.cumsum(padded)
    pstarts = pends - padded
    dest = pstarts[se] + jnp.arange(NK, dtype=jnp.int32) - starts[se]
    n_blocks = -(-NK // MOE_BLOCK) + N_EXPERTS
    cap = n_blocks * MOE_BLOCK
    tok_buf = jnp.full((cap,), N, jnp.int32).at[dest].set(stok)
    w_buf = jnp.zeros((cap,), f32).at[dest].set(sw)
    block_e = jnp.minimum(
        jnp.searchsorted(pends, jnp.arange(n_blocks, dtype=jnp.int32) * MOE_BLOCK, side='right'),
        N_EXPERTS - 1).astype(jnp.int32)
    x_pad = jnp.concatenate([xt, jnp.zeros((1, D_), xt.dtype)], axis=0)
    xb = x_pad[tok_buf].reshape(n_blocks, MOE_BLOCK, D_)

    def expert_block(args):
        xblk, e = args
        gu = xblk @ w_gu[e] + b_gu[e]
        g, up = gu[:, :D_FF], gu[:, D_FF:]
        g = jnp.minimum(g, SWIGLU_LIMIT)
        up = jnp.clip(up, -SWIGLU_LIMIT, SWIGLU_LIMIT)
        act = (up + 1.0) * (g * jax.nn.sigmoid(SWIGLU_ALPHA * g))
        return act @ w_down[e] + b_down[e]

    yb = lax.map(expert_block, (xb, block_e)).reshape(cap, D_)
    out = jnp.zeros((N + 1, D_), f32).at[tok_buf].add(yb.astype(f32) * w_buf[:, None])[:N]
    return out.reshape(B_, T_, D_).astype(h.dtype)


def setup_inputs(seed: int = 0) -> dict:
    key = jax.random.key(seed)
    ks = jax.random.split(key, 32)
    f32 = jnp.float32
    L, D = DEPTH, D_MODEL

    def nrm(k, shape, scale):
        return jax.random.normal(k, shape, f32) * scale

    n_idx = jnp.arange(S5_STATE, dtype=f32)
    return {
        "x": nrm(ks[0], (BATCH, SEQ, D), 1.0),
        "c": nrm(ks[1], (BATCH, D), 1.0),
        "ada_w": nrm(ks[2], (L, D, 6 * D), 0.02),
        "ada_b": nrm(ks[3], (L, 6 * D), 0.02),
        "mix_pre_g": 1.0 + nrm(ks[4], (L, D), 0.05),
        "mix_post_g": 1.0 + nrm(ks[5], (L, D), 0.05),
        "ffn_pre_g": 1.0 + nrm(ks[6], (L, D), 0.05),
        "ffn_post_g": 1.0 + nrm(ks[7], (L, D), 0.05),
        "w_in": nrm(ks[8], (L, D, N_IN), D ** -0.5),
        "w_alpha": nrm(ks[9], (L, GLA_LOWRANK, GLA_HEADS * GLA_DK), GLA_LOWRANK ** -0.5),
        "b_alpha": nrm(ks[10], (L, GLA_HEADS * GLA_DK), 0.1),
        "gla_norm_g": 1.0 + nrm(ks[11], (L, GLA_DV), 0.05),
        "s5_lambda_re": -0.5 + nrm(ks[12], (L, S5_GROUPS, S5_STATE), 0.01),
        "s5_lambda_im": math.pi * n_idx + nrm(ks[13], (L, S5_GROUPS, S5_STATE), 0.01),
        "s5_log_dt": jax.random.uniform(ks[14], (L, S5_GROUPS), f32, math.log(DT_MIN), math.log(DT_MAX)),
        "s5_b_re": nrm(ks[15], (L, S5_GROUPS, S5_STATE, S5_CH), (2 * S5_CH) ** -0.5),
        "s5_b_im": nrm(ks[16], (L, S5_GROUPS, S5_STATE, S5_CH), (2 * S5_CH) ** -0.5),
        "s5_c_re": nrm(ks[17], (L, S5_GROUPS, S5_CH, S5_STATE), S5_STATE ** -0.5),
        "s5_c_im": nrm(ks[18], (L, S5_GROUPS, S5_CH, S5_STATE), S5_STATE ** -0.5),
        "s5_d": nrm(ks[19], (L, S5_GROUPS, S5_CH), 1.0),
        "s5_glu_w": nrm(ks[20], (L, S5_WIDTH, S5_WIDTH), S5_WIDTH ** -0.5),
        "s5_glu_b": nrm(ks[21], (L, S5_WIDTH), 0.01),
        "w_out": nrm(ks[22], (L, D_MIX, D), D_MIX ** -0.5),
        "router_w": nrm(ks[23], (L, D, N_EXPERTS), D ** -0.5),
        "router_b": nrm(ks[24], (L, N_EXPERTS), 0.01),
        "exp_w_gu": nrm(ks[25], (L, N_EXPERTS, D, 2 * D_FF), D ** -0.5),
        "exp_b_gu": nrm(ks[26], (L, N_EXPERTS, 2 * D_FF), 0.01),
        "exp_w_down": nrm(ks[27], (L, N_EXPERTS, D_FF, D), D_FF ** -0.5),
        "exp_b_down": nrm(ks[28], (L, N_EXPERTS, D), 0.01),
    }


def reference(x, c, ada_w, ada_b, mix_pre_g, mix_post_g, ffn_pre_g, ffn_post_g, w_in, w_alpha, b_alpha,
              gla_norm_g, s5_lambda_re, s5_lambda_im, s5_log_dt, s5_b_re, s5_b_im, s5_c_re, s5_c_im, s5_d,
              s5_glu_w, s5_glu_b, w_out, router_w, router_b, exp_w_gu, exp_b_gu, exp_w_down, exp_b_down):
    B_, T_, D_ = x.shape
    h = x
    for l in range(DEPTH):
        mod = jax.nn.silu(c) @ ada_w[l] + ada_b[l]
        sh1, sc1, g1, sh2, sc2, g2 = jnp.split(mod, 6, axis=-1)

        hn = rmsnorm(h, mix_pre_g[l]) * (1.0 + sc1[:, None, :]) + sh1[:, None, :]
        proj = hn @ w_in[l]
        q, k, v, og, a_lr, u = jnp.split(proj, SPLITS, axis=-1)

        def heads(t, dh):
            return t.reshape(B_, T_, GLA_HEADS, dh).transpose(0, 2, 1, 3)

        log_a = jax.nn.log_sigmoid((a_lr @ w_alpha[l] + b_alpha[l]).astype(jnp.float32)) / GLA_TAU
        o = gla_chunked(heads(q, GLA_DK), heads(k, GLA_DK), heads(v, GLA_DV), heads(log_a, GLA_DK))
        o = rmsnorm(o, gla_norm_g[l]).transpose(0, 2, 1, 3)
        gla_out = (o * jax.nn.silu(og.astype(jnp.float32)).reshape(B_, T_, GLA_HEADS, GLA_DV))
        gla_out = gla_out.reshape(B_, T_, GLA_WIDTH).astype(hn.dtype)

        s5_out = s5_layer(u, s5_lambda_re[l], s5_lambda_im[l], s5_log_dt[l], s5_b_re[l], s5_b_im[l],
                          s5_c_re[l], s5_c_im[l], s5_d[l], s5_glu_w[l], s5_glu_b[l]).astype(hn.dtype)

        mix = jnp.concatenate([gla_out, s5_out], axis=-1) @ w_out[l]
        h = h + g1[:, None, :] * rmsnorm(mix, mix_post_g[l])

        hn2 = rmsnorm(h, ffn_pre_g[l]) * (1.0 + sc2[:, None, :]) + sh2[:, None, :]
        ff = moe_ffn(hn2, router_w[l], router_b[l], exp_w_gu[l], exp_b_gu[l], exp_w_down[l], exp_b_down[l])
        h = h + g2[:, None, :] * rmsnorm(ff, ffn_post_g[l])
    return h
```

```python
import math
from contextlib import ExitStack
import numpy as np
import concourse.bass as bass
import concourse.mybir as mybir
from concourse.bass_utils import run_bass_kernel_spmd

F32 = mybir.dt.float32
BF16 = mybir.dt.bfloat16
I32 = mybir.dt.int32
AF = mybir.ActivationFunctionType
ALU = mybir.AluOpType

D = 1024
T = 2048
NT = T // 128
NE = 32
EPS = 1e-6
PHASES = ("ada", "mixer", "moe")


class Prog:
    def __init__(self, nc, same_engine_sync=True, ndma_sems=12):
        self.nc = nc
        self.names = ('pe', 'act', 'dve', 'pool', 'sp')
        self.q = {e: [] for e in self.names}
        self.cnt = {e: 0 for e in self.names}
        self.sem = {}
        self.dsem = {}
        self.dcnt = {e: 0 for e in self.names}
        self.ndma = ndma_sems
        self.last_w = {}
        self.readers = {}
        self.waited = {e: {} for e in self.names}
        self.same = same_engine_sync
        self.recent_dma = {e: [] for e in self.names}
        self.last_tok = {}
        self.gcnt = 0
        self.gtoks = []
        self.gdma = 6

    def alloc_sems(self, stack):
        for e in self.names:
            self.sem[e] = stack.enter_context(self.nc.semaphore(f"s_{e}"))
        for e in ('sp', 'pool', 'act', 'grp'):
            self.dsem[e] = [stack.enter_context(self.nc.semaphore(f"d_{e}{i}")) for i in range(self.ndma)]

    def _deps(self, r, w):
        toks = []
        for k in r:
            if k in self.last_w:
                toks.append(self.last_w[k])
        for k in w:
            if k in self.last_w:
                toks.append(self.last_w[k])
            toks.extend(self.readers.get(k, []))
        return toks

    def _waits(self, e, toks):
        need = {}
        for (skey, val, src_e, kind) in toks:
            if kind == 'nosig' and src_e == e:
                continue
            if kind != 'dma' and src_e == e and not self.same:
                continue
            if self.waited[e].get(skey, 0) >= val:
                continue
            if need.get(skey, 0) < val:
                need[skey] = val
        out = []
        for skey, val in need.items():
            self.waited[e][skey] = val
            out.append((skey, val))
        return out

    def _semobj(self, skey):
        if skey[0] == 'c':
            return self.sem[skey[1]]
        return self.dsem[skey[1]][skey[2]]

    def _record(self, tok, r, w):
        for k in r:
            self.readers.setdefault(k, []).append(tok)
        for k in w:
            self.last_w[k] = tok
            self.readers[k] = []

    def op(self, e, fn, r=(), w=(), sig=True):
        toks = self._deps(r, w)
        waits = self._waits(e, toks)
        if sig:
            self.cnt[e] += 1
            idx = self.cnt[e]
        else:
            idx = self.cnt[e] + 1
        tok = (('c', e), idx, e, 'sig' if sig else 'nosig')
        self.q[e].append((fn, waits, (('c', e), 1) if sig else None))
        self._record(tok, r, w)
        if sig:
            self.last_tok[e] = tok
        return tok

    def dma(self, e, fn, r=(), w=(), grp=None):
        toks = self._deps(r, w)
        if grp is None:
            j = self.dcnt[e]
            self.dcnt[e] += 1
            qn = e
        else:
            j = self.gcnt
            self.gcnt += 1
            qn = 'grp'
        nd = self.ndma if grp is None else self.gdma
        slot = j % nd
        val = 16 * (j // nd + 1)
        skey = ('d', qn, slot)
        if j >= nd:
            toks.append((skey, val - 16, e, 'dma'))
        waits = self._waits(e, toks)
        tok = (skey, val, e, 'dma')
        self.q[e].append((fn, waits, (skey, 16)))
        self._record(tok, r, w)
        if grp is None:
            self.recent_dma[e].append(tok)
            self.recent_dma[e] = self.recent_dma[e][-self.ndma:]
        else:
            self.gtoks.append(tok)
            self.gtoks = self.gtoks[-self.gdma:]
        return tok

    def wait_group(self, engines):
        for e in engines:
            waits = self._waits(e, list(self.gtoks))
            self.q[e].append((None, waits, None))

    def last_w_merge(self, key, other):
        pass

    def barrier(self):
        toks = [t for t in self.last_tok.values()]
        for e in self.names:
            toks.extend(self.recent_dma[e])
        for e in self.names:
            waits = self._waits(e, [t for t in toks if not (t[3] != 'dma' and t[2] == e)])
            self.q[e].append((None, waits, None))
        self.last_w = {}
        self.readers = {}

    def emit(self, block):
        prog = self

        def run(e):
            items = prog.q[e]
            prog.q[e] = []

            def body(engine):
                for fn, waits, inc in items:
                    for skey, val in waits:
                        engine.wait_ge(prog._semobj(skey), val)
                    if fn is None:
                        continue
                    ins = fn(engine)
                    if inc is not None:
                        ins.then_inc(prog._semobj(inc[0]), inc[1])
            return body
        block.tensor(run('pe'))
        block.scalar(run('act'))
        block.vector(run('dve'))
        block.gpsimd(run('pool'))
        block.sync(run('sp'))


def build(debug=False, phases=PHASES, n_exp=NE):
    nc = bass.Bass("TRN2", target_bir_lowering=False)

    def din(name, shape, dt=F32):
        return nc.dram_tensor(name, list(shape), dt, kind="ExternalInput").ap()

    x_d = din("x", [T, D])
    c_d = din("c128", [128, 8])
    adaw_d = din("ada_w", [D, 6 * D])
    adab_col_d = din("ada_b_col", [128, 48])
    adab_row_d = din("ada_b_row", [1, 6 * D])
    preg1_d = din("mix_pre_g", [128, 8])
    preg2_d = din("ffn_pre_g", [128, 8])
    postg1_d = din("mix_post_g", [1, D])
    postg2_d = din("ffn_post_g", [1, D])
    win_d = din("w_in", [D, 2064])
    walpha_d = din("w_alpha", [16, 256])
    balpha_d = din("b_alpha", [64, 4])
    glag_d = din("gla_norm_g", [128, 1])
    lre_d = din("s5_lre", [128, 16])
    lim_d = din("s5_lim", [128, 16])
    ldt_d = din("s5_ldt", [128, 16])
    bre_d = din("s5_bre", [128, 16, 16])
    bim_d = din("s5_bim", [128, 16, 16])
    cre_d = din("s5_cre", [128, 16, 16])
    cim_d = din("s5_cim", [128, 16, 16])
    s5d_d = din("s5_d", [128, 4])
    gluw_d = din("s5_glu_w", [512, 512])
    glub_d = din("s5_glu_b", [128, 4])
    wout_d = din("w_out", [D, D])
    rw_d = din("router_w", [D, NE])
    rb_d = din("router_b", [1, NE])
    wgu_d = din("exp_w_gu", [NE, D, 2 * D])
    bgu_d = din("exp_b_gu", [128, NE, 16])
    wdn_d = din("exp_w_down", [NE, D, D])
    bdn_d = din("exp_b_down", [NE, D])
    y_d = nc.dram_tensor("y", [T, D], F32, kind="ExternalOutput").ap()
    wgb_d = nc.dram_tensor("wgb_scratch", [NE * 128, 8, 2 * D], BF16).ap()
    wdb_d = nc.dram_tensor("wdb_scratch", [NE * 128, 8, D], BF16).ap()
    dbg = {}
    if debug:
        for nm, shp in (("dbg_modc", [128, 32]), ("dbg_gp", [128, 2048]), ("dbg_hn", [128, 8 * T]),
                        ("dbg_mixin", [128, 8 * T]), ("dbg_h", [T, D]), ("dbg_G", [128, NT * NE]),
                        ("dbg_acc", [128, NT * D])):
            dbg[nm] = nc.dram_tensor(nm, shp, F32, kind="ExternalOutput").ap()

    with ExitStack() as st:
        P = Prog(nc)

        _names = {}

        def sb(name, shape, dt=F32, stack=None):
            _names[name] = _names.get(name, 0) + 1
            if _names[name] > 1:
                name = f"{name}_v{_names[name]}"
            return (stack or st).enter_context(nc.sbuf_tensor(name, list(shape), dt))

        ident = sb("ident", [128, 128])
        identb = sb("identb", [128, 128], BF16)
        ones_b = sb("ones_b", [128, 128], BF16)
        S1 = sb("S1", [128, 8]); B1 = sb("B1", [128, 8]); S2 = sb("S2", [128, 8]); B2 = sb("B2", [128, 8])
        GP2 = sb("GP2", [128, D])
        gp1_stack = ExitStack()
        GP1 = sb("GP1", [128, D], stack=gp1_stack)
        pbanks = [st.enter_context(nc.psum_tensor(f"pb{i}", [128, 512], F32)) for i in range(8)]
        P.alloc_sems(st)
        block = st.enter_context(nc.Block())

        P.op('pool', lambda e: e.memset(ident[:], 0.0), w=['ident'])
        P.op('pool', lambda e: e.affine_select(out=ident[:], in_=ident[:], pattern=[[-1, 128]], compare_op=ALU.not_equal,
                                               fill=1.0, base=0, channel_multiplier=1), r=['ident'], w=['ident'])
        P.op('dve', lambda e: e.tensor_copy(out=identb[:], in_=ident[:]), r=['ident'], w=['identb'])
        P.op('dve', lambda e: e.memset(ones_b[:], 1.0), w=['ones_b'])

        xs_d = nc.dram_tensor("xs_scratch", [63 * 256, D], BF16).ap()

        def dbg_dump(name, src_ap, keys, cols=None):
            if not debug:
                return
            d = dbg[name] if cols is None else dbg[name][:, cols[0]:cols[1]]
            P.dma('sp', lambda e: e.dma_start(out=d, in_=src_ap), r=keys)

        def issue_cast_pass(e_lo=0, e_hi=NE):
            wgr = wgu_d.rearrange("e r n -> (e r) n")
            wdr = wdn_d.rearrange("e r n -> (e r) n")
            for ei in range(e_lo, e_hi):
                for kc in range(8):
                    rs = slice((ei * 8 + kc) * 128, (ei * 8 + kc + 1) * 128)
                    ds_ = slice(ei * 128, (ei + 1) * 128)
                    P.dma('pool', lambda e, rs=rs, ds_=ds_, kc=kc: e.dma_start(out=wgb_d[ds_, kc, :], in_=wgr[rs, :]), grp='cast')
                    P.dma('pool', lambda e, rs=rs, ds_=ds_, kc=kc: e.dma_start(out=wdb_d[ds_, kc, :], in_=wdr[rs, :]), grp='cast')

        with ExitStack() as pa:
            c_t = sb("c_t", [128, 8], stack=pa)
            sc_t = sb("sc_t", [128, 8], stack=pa)
            scb = sb("scb", [128, 8, 128], stack=pa)
            adab_c = sb("adab_c", [128, 48], stack=pa)
            pg1 = sb("pg1", [128, 8], stack=pa); pg2 = sb("pg2", [128, 8], stack=pa)
            abr = sb("abr", [128, 2, D], stack=pa)
            pgr = sb("pgr", [128, 2, D], stack=pa)
            modc = sb("modc", [128, 32], stack=pa)
            aw = [sb(f"aw{i}", [128, 6 * D], stack=pa) for i in range(2)]
            ZT = sb("ZT", [128, D], BF16, stack=pa)
            P.op('dve', lambda e: e.memset(ZT[:], 0.0), w=['ZT'])
            xs_v0 = xs_d.rearrange("(a p) d -> p a d", p=128)
            for a9 in range(14):
                P.dma('act', lambda e, a9=a9: e.dma_start(out=xs_v0[:, a9 * 9:(a9 + 1) * 9, :], in_=ZT[:].unsqueeze(1).to_broadcast([128, 9, D])),
                      r=['ZT'], w=[('xsz', a9)])
            P.dma('sp', lambda e: e.dma_start(out=c_t[:], in_=c_d[:, :]), w=['c_t'])
            P.dma('sp', lambda e: e.dma_start(out=adab_c[:], in_=adab_col_d[:, :]), w=['adab_c'])
            P.dma('sp', lambda e: e.dma_start(out=pg1[:], in_=preg1_d[:, :]), w=['pg1'])
            P.dma('sp', lambda e: e.dma_start(out=pg2[:], in_=preg2_d[:, :]), w=['pg2'])
            P.dma('pool', lambda e: e.dma_start(out=abr[:, 0, :], in_=adab_row_d[0:1, 2 * D:3 * D].partition_broadcast(128)), w=['abr0'])
            P.dma('pool', lambda e: e.dma_start(out=abr[:, 1, :], in_=adab_row_d[0:1, 5 * D:6 * D].partition_broadcast(128)), w=['abr1'])
            P.dma('pool', lambda e: e.dma_start(out=pgr[:, 0, :], in_=postg1_d[0:1, :].partition_broadcast(128)), w=['pgr0'])
            P.dma('pool', lambda e: e.dma_start(out=pgr[:, 1, :], in_=postg2_d[0:1, :].partition_broadcast(128)), w=['pgr1'])
            P.op('act', lambda e: e.activation(out=sc_t[:], in_=c_t[:], func=AF.Silu), r=['c_t'], w=['sc_t'])
            P.op('dve', lambda e: e.tensor_copy(out=scb[:], in_=sc_t[:].unsqueeze(2).to_broadcast([128, 8, 128])), r=['sc_t'], w=['scb'])
            colchunks = list(range(0, 16)) + list(range(24, 40))
            pcol = pbanks[0]
            prow = pbanks[1:5]
            first = True
            for kc in range(8):
                b = kc % 2
                P.dma('sp', lambda e, kc=kc, b=b: e.dma_start(out=aw[b][:], in_=adaw_d[kc * 128:(kc + 1) * 128, :]), w=[f'aw{b}'])
                for i, m in enumerate(colchunks):
                    last = (kc == 7 and i == len(colchunks) - 1)
                    P.op('pe', lambda e, b=b, m=m, i=i, kc=kc, first=first, last=last: e.matmul(
                        pcol[:, i:i + 1], aw[b][:, m * 128:(m + 1) * 128], sc_t[:, kc:kc + 1], start=first, stop=last),
                        r=[f'aw{b}', 'sc_t'], w=['pcol'], sig=(i == len(colchunks) - 1))
                    first = False
                for q in range(4):
                    col0 = (2 * D if q < 2 else 5 * D) + (q % 2) * 512
                    P.op('pe', lambda e, b=b, q=q, kc=kc, col0=col0: e.matmul(
                        prow[q][:], scb[:, kc, :], aw[b][:, col0:col0 + 512], start=(kc == 0), stop=(kc == 7)),
                        r=[f'aw{b}', 'scb'], w=[f'prow{q}'])
            P.op('dve', lambda e: e.tensor_tensor(out=modc[:, 0:16], in0=pcol[:, 0:16], in1=adab_c[:, 0:16], op=ALU.add),
                 r=['pcol', 'adab_c'], w=['modc'])
            P.op('dve', lambda e: e.tensor_tensor(out=modc[:, 16:32], in0=pcol[:, 16:32], in1=adab_c[:, 24:40], op=ALU.add),
                 r=['pcol', 'adab_c'], w=['modc'])
            P.op('dve', lambda e: e.scalar_tensor_tensor(out=S1[:], in0=modc[:, 8:16], scalar=1.0, in1=pg1[:], op0=ALU.add, op1=ALU.mult),
                 r=['modc', 'pg1'], w=['S1'])
            P.op('dve', lambda e: e.tensor_copy(out=B1[:], in_=modc[:, 0:8]), r=['modc'], w=['B1'])
            P.op('dve', lambda e: e.scalar_tensor_tensor(out=S2[:], in0=modc[:, 24:32], scalar=1.0, in1=pg2[:], op0=ALU.add, op1=ALU.mult),
                 r=['modc', 'pg2'], w=['S2'])
            P.op('dve', lambda e: e.tensor_copy(out=B2[:], in_=modc[:, 16:24]), r=['modc'], w=['B2'])
            for q in range(4):
                gp = GP1 if q < 2 else GP2
                j = q // 2
                sl = slice((q % 2) * 512, (q % 2) * 512 + 512)
                P.op('dve', lambda e, gp=gp, j=j, sl=sl, q=q: e.tensor_tensor(out=gp[:, sl], in0=prow[q][:], in1=abr[:, j, sl], op=ALU.add),
                     r=[f'prow{q}', f'abr{j}'], w=[f'gp{q}'])
                P.op('dve', lambda e, gp=gp, j=j, sl=sl, q=q: e.tensor_tensor(out=gp[:, sl], in0=gp[:, sl], in1=pgr[:, j, sl], op=ALU.mult),
                     r=[f'gp{q}', f'pgr{j}'], w=[f'gp{q}'])
            if debug:
                dbg_dump("dbg_modc", modc[:], ['modc'])
                dbg_dump("dbg_gp", GP1[:], ['gp0', 'gp1'], cols=(0, 1024))
                dbg_dump("dbg_gp", GP2[:], ['gp2', 'gp3'], cols=(1024, 2048))
            P.barrier()
            P.emit(block)

        def prenorm_tile(src, src_keys, ti, HN, hn_key, Sc, Bc, tmp, pst, f32copy=None, sx=''):
            ssq, rr, xs = tmp
            P.op('act', lambda e: e.activation(out=xs[:], in_=src, func=AF.Square, accum_out=ssq[:]), r=src_keys, w=['pn_xs' + sx, 'pn_ss' + sx])
            P.op('act', lambda e: e.activation(out=rr[:], in_=ssq[:], func=AF.Ln, scale=1.0 / D, bias=EPS), r=['pn_ss' + sx], w=['pn_rr' + sx])
            P.op('act', lambda e: e.activation(out=rr[:], in_=rr[:], func=AF.Exp, scale=-0.5), r=['pn_rr' + sx], w=['pn_rr' + sx])
            P.op('dve', lambda e: e.tensor_scalar(out=xs[:], in0=src, scalar1=rr[:, 0:1], scalar2=None, op0=ALU.mult), r=src_keys + ['pn_rr' + sx], w=['pn_xs' + sx])
            for half in range(2):
                pt = pst[half]
                for q in range(4):
                    kc = half * 4 + q
                    P.op('pe', lambda e, pt=pt, q=q, kc=kc: e.transpose(out=pt[:, q * 128:(q + 1) * 128], in_=xs[:, kc * 128:(kc + 1) * 128], identity=ident[:]),
                         r=['pn_xs' + sx, 'ident'], w=[f'pb{half}'], sig=(q == 3))
                for q in range(4):
                    kc = half * 4 + q
                    if HN is not None:
                        P.op('act', lambda e, pt=pt, q=q, kc=kc: e.activation(out=HN[:, kc, ti * 128:(ti + 1) * 128], in_=pt[:, q * 128:(q + 1) * 128],
                                                                              func=AF.Identity, scale=Sc[:, kc:kc + 1], bias=Bc[:, kc:kc + 1]),
                             r=[f'pb{half}'], w=[hn_key(kc, ti)])
                    if f32copy is not None:
                        P.op('act', lambda e, pt=pt, q=q, kc=kc: e.activation(out=f32copy[:, kc, :], in_=pt[:, q * 128:(q + 1) * 128],
                                                                              func=AF.Identity, scale=Sc[:, kc:kc + 1], bias=Bc[:, kc:kc + 1]),
                             r=[f'pb{half}'], w=['pn_f32' + sx])

        build_rest(nc, P, st, block, locals(), debug, phases, n_exp)
    return nc


def build_rest(nc, P, st, block, L, debug, phases, n_exp):
    sb = L['sb']; pbanks = L['pbanks']; ident = L['ident']; identb = L['identb']; ones_b = L['ones_b']
    S1, B1, S2, B2, GP1, GP2 = L['S1'], L['B1'], L['S2'], L['B2'], L['GP1'], L['GP2']
    x_d, y_d = L['x_d'], L['y_d']
    dbg = L['dbg']; dbg_dump = L['dbg_dump']; prenorm_tile = L['prenorm_tile']
    rw_d, rb_d, wgu_d, bgu_d, wdn_d, bdn_d = L['rw_d'], L['rb_d'], L['wgu_d'], L['bgu_d'], L['wdn_d'], L['bdn_d']

    if "mixer" in phases:
        import_mixer = build_mixer
        import_mixer(nc, P, st, block, L, debug)
    else:
        L['issue_cast_pass'](0, NE)
        with ExitStack() as pb:
            xt = sb("xt_cp", [128, D], stack=pb)
            for ti in range(NT):
                P.dma('sp', lambda e, ti=ti: e.dma_start(out=xt[:], in_=x_d[ti * 128:(ti + 1) * 128, :]), w=['xt_cp'])
                P.dma('sp', lambda e, ti=ti: e.dma_start(out=y_d[ti * 128:(ti + 1) * 128, :], in_=xt[:]), r=['xt_cp'], w=[('y', ti)])
            P.barrier()
            P.emit(block)

    L['gp1_stack'].close()
    if 'moe' not in phases:
        return
    NB = 63
    BS = 256
    NSLOT = NB * BS
    X = mybir.AxisListType.X
    xs_d = L['xs_d']
    assert NSLOT == 63 * 256
    ys_d = nc.dram_tensor("ys_scratch", [NSLOT, D], F32).ap()
    wgu_rows = L['wgb_d']
    wdn_rows = L['wdb_d']
    bgu_rows = bgu_d.rearrange("p e c -> (p e) c")
    IOA = bass.IndirectOffsetOnAxis
    with ExitStack() as pc:
        G = sb("G", [128, NT, NE], stack=pc)
        SLI = sb("SLI", [128, NT, 4], I32, stack=pc)
        GK = sb("GK", [128, NT, 4], stack=pc)
        OFFW = sb("OFFW", [128, NB], I32, stack=pc)
        OFFB = sb("OFFB", [128, NB], I32, stack=pc)
        BD = sb("BD", [NE, D], stack=pc)
        P.dma('sp', lambda e: e.dma_start(out=BD[:], in_=bdn_d[:, :]), w=['BD'])

        with ExitStack() as c1:
            HR = sb("HR", [128, NT, D], BF16, stack=c1)
            RW = sb("RW", [128, 8, NE], stack=c1)
            RB = sb("RB", [128, NE], stack=c1)
            S2R = sb("S2R", [128, D], stack=c1)
            B2R = sb("B2R", [128, D], stack=c1)
            bct = sb("bct", [128, 128], stack=c1)
            MK = sb("MK", [128, NT, NE], BF16, stack=c1)
            LT = sb("LT", [128, 128], stack=c1)
            LTb = sb("LTb", [128, 128], BF16, stack=c1)
            POS = sb("POS", [128, NT, NE], stack=c1)
            SM = sb("SM", [128, NT, NE], stack=c1)
            CNT = sb("CNT", [128, NE], stack=c1)
            NBK = sb("NBK", [128, NE], stack=c1)
            NBI = sb("NBI", [128, NE], I32, stack=c1)
            tq = sb("tq", [128, NE], stack=c1)
            CS = sb("CS", [128, NE], stack=c1)
            PST = sb("PST", [128, NE], stack=c1)
            PEND = sb("PEND", [128, NE], stack=c1)
            one32 = sb("one32", [128, NE], stack=c1)
            SL4 = sb("SL4", [128, NT, 4], stack=c1)
            eq4 = sb("eq4", [128, 4, NE], stack=c1)
            BTHI = sb("BTHI", [128, NB, NE], I32, stack=c1)
            BTH = sb("BTH", [128, NB, NE], stack=c1)
            EB = sb("EB", [128, NB], stack=c1)
            SK = sb("SK", [128, NB], stack=c1)
            KCI = sb("KCI", [128, NB], I32, stack=c1)
            KCF = sb("KCF", [128, NB], stack=c1)
            PI = sb("PI", [128, NB], I32, stack=c1)
            PF = sb("PF", [128, NB], stack=c1)
            P.dma('sp', lambda e: e.dma_start(out=RW[:], in_=rw_d.rearrange("(kc p) n -> p kc n", p=128)), w=['RW'])
            P.dma('pool', lambda e: e.dma_start(out=RB[:], in_=rb_d[0:1, :].partition_broadcast(128)), w=['RB'])
            pr = [pbanks[6], pbanks[7]]
            for vi, (col, rowt, nm) in enumerate(((S2, S2R, 'S2R'), (B2, B2R, 'B2R'))):
                for kc in range(8):
                    P.op('dve', lambda e, col=col, kc=kc: e.tensor_copy(out=bct[:], in_=col[:, kc:kc + 1].to_broadcast([128, 128])), w=['bct'])
                    bank = pr[kc // 4]
                    P.op('pe', lambda e, bank=bank, kc=kc: e.matmul(bank[:, (kc % 4) * 128:(kc % 4 + 1) * 128], bct[:], ident[:], start=True, stop=True),
                         r=['bct', 'ident'], w=[f'pb{6 + kc // 4}'])
                    if kc % 4 == 3:
                        h_ = kc // 4
                        P.op('act', lambda e, bank=bank, rowt=rowt, h_=h_: e.activation(out=rowt[:, h_ * 512:(h_ + 1) * 512], in_=bank[:], func=AF.Identity),
                             r=[f'pb{6 + h_}'], w=[nm])
            pst = [pbanks[0], pbanks[1]]
            plg = pbanks[2]
            dbl = {}
            for nm_, shp_ in (("ht", [128, D]), ("pn_ss", [128, 1]), ("pn_rr", [128, 1]), ("pn_xs", [128, D]), ("hn32", [128, 8, 128]), ("lg", [128, NE]),
                              ("top8", [128, 8]), ("nm1", [128, 1]), ("msk", [128, NE]), ("ex", [128, NE]), ("ssum", [128, 1])):
                dbl[nm_] = [sb(nm_ + "_d%d" % i, shp_, stack=c1) for i in range(4 if nm_ == "ht" else 2)]
            for ti in range(NT):
                k2 = ti % 2
                sx = str(k2)
                ht_, ssq_, rr_, xs_, hn32_, lg_, top8_, nm1_, msk_, ex_, ssum_ = [dbl[n][k2] for n in ("ht", "pn_ss", "pn_rr", "pn_xs", "hn32", "lg", "top8", "nm1", "msk", "ex", "ssum")]
                ht_ = dbl["ht"][ti % 4]
                hx = 'h' + str(ti % 4)
                P.dma('sp', lambda e, ti=ti, ht_=ht_: e.dma_start(out=ht_[:], in_=y_d[ti * 128:(ti + 1) * 128, :]), r=[('y', ti)], w=['ht' + hx])
                prenorm_tile(ht_[:], ['ht' + hx], ti, None, None, S2, B2, (ssq_, rr_, xs_), pst, f32copy=hn32_, sx=sx)
                P.op('dve', lambda e, ht_=ht_, xs_=xs_: e.tensor_tensor(out=ht_[:], in0=xs_[:], in1=S2R[:], op=ALU.mult), r=['pn_xs' + sx, 'S2R'], w=['ht' + hx])
                P.op('dve', lambda e, ti=ti, ht_=ht_: e.tensor_tensor(out=HR[:, ti, :], in0=ht_[:], in1=B2R[:], op=ALU.add), r=['ht' + hx, 'B2R'], w=[('HR', ti)])
                for kc in range(8):
                    P.op('pe', lambda e, kc=kc, hn32_=hn32_: e.matmul(plg[:, 0:NE], hn32_[:, kc, :], RW[:, kc, :], start=(kc == 0), stop=(kc == 7)),
                         r=['pn_f32' + sx, 'RW'], w=['plg'], sig=(kc == 7))
                P.op('dve', lambda e, lg_=lg_: e.tensor_tensor(out=lg_[:], in0=plg[:, 0:NE], in1=RB[:], op=ALU.add), r=['plg', 'RB'], w=['lg' + sx])
                P.op('dve', lambda e, lg_=lg_, top8_=top8_: e.max(out=top8_[:], in_=lg_[:]), r=['lg' + sx], w=['top8' + sx])
                P.op('dve', lambda e, top8_=top8_, nm1_=nm1_: e.tensor_scalar(out=nm1_[:], in0=top8_[:, 0:1], scalar1=-1.0, scalar2=None, op0=ALU.mult), r=['top8' + sx], w=['nm1' + sx])
                P.op('dve', lambda e, lg_=lg_, top8_=top8_, msk_=msk_: e.tensor_scalar(out=msk_[:], in0=lg_[:], scalar1=top8_[:, 3:4], scalar2=None, op0=ALU.is_ge),
                     r=['lg' + sx, 'top8' + sx], w=['msk' + sx])
                P.op('dve', lambda e, ti=ti, msk_=msk_: e.tensor_copy(out=MK[:, ti, :], in_=msk_[:]), r=['msk' + sx], w=[('MK', ti)])
                P.op('act', lambda e, lg_=lg_, nm1_=nm1_, ex_=ex_: e.activation(out=ex_[:], in_=lg_[:], func=AF.Exp, bias=nm1_[:, 0:1], scale=1.0), r=['lg' + sx, 'nm1' + sx], w=['ex' + sx])
                P.op('dve', lambda e, ex_=ex_, msk_=msk_: e.tensor_tensor(out=ex_[:], in0=ex_[:], in1=msk_[:], op=ALU.mult), r=['ex' + sx, 'msk' + sx], w=['ex' + sx])
                P.op('dve', lambda e, ex_=ex_, ssum_=ssum_: e.reduce_sum(out=ssum_[:], in_=ex_[:], axis=X), r=['ex' + sx], w=['ssum' + sx])
                P.op('dve', lambda e, ssum_=ssum_: e.reciprocal(out=ssum_[:], in_=ssum_[:]), r=['ssum' + sx], w=['ssum' + sx])
                P.op('dve', lambda e, ti=ti, ex_=ex_, ssum_=ssum_: e.tensor_scalar(out=G[:, ti, :], in0=ex_[:], scalar1=ssum_[:, 0:1], scalar2=None, op0=ALU.mult),
                     r=['ex' + sx, 'ssum' + sx], w=[('G', ti)])
            if debug:
                dbg_dump("dbg_G", G[:].rearrange("p a b -> p (a b)"), [('G', ti) for ti in range(NT)])
            top8 = dbl['top8'][0]
            GKEYS = [('G', ti) for ti in range(NT)]
            MKEYS = [('MK', ti) for ti in range(NT)]
            P.op('pool', lambda e: e.memset(LT[:], 1.0), w=['LT'])
            P.op('pool', lambda e: e.affine_select(out=LT[:], in_=LT[:], pattern=[[1, 128]], compare_op=ALU.is_ge, fill=0.0, base=-1, channel_multiplier=-1),
                 r=['LT'], w=['LT'])
            P.op('dve', lambda e: e.tensor_copy(out=LTb[:], in_=LT[:]), r=['LT'], w=['LTb'])
            ppos = pbanks[3]
            pcnt = pbanks[4]
            for ti in range(NT):
                for tj in range(ti + 1):
                    lt = ones_b if tj < ti else LTb
                    P.op('pe', lambda e, ti=ti, tj=tj, lt=lt: e.matmul(ppos[:, ti * NE:(ti + 1) * NE], lt[:], MK[:, tj, :], start=(tj == 0), stop=(tj == ti)),
                         r=[('MK', tj), 'LTb', 'ones_b'], w=['pb3'], sig=(tj == ti))
            for tj in range(NT):
                P.op('pe', lambda e, tj=tj: e.matmul(pcnt[:, 0:NE], ones_b[:], MK[:, tj, :], start=(tj == 0), stop=(tj == NT - 1)),
                     r=[('MK', tj), 'ones_b'], w=['pb4'], sig=(tj == NT - 1))
            dv = lambda fn, r, w: P.op('dve', fn, r=r, w=w)
            dv(lambda e: e.tensor_copy(out=POS[:].rearrange("p a b -> p (a b)"), in_=ppos[:]), ['pb3'], ['POS'])
            dv(lambda e: e.tensor_copy(out=CNT[:], in_=pcnt[:, 0:NE]), ['pb4'], ['CNT'])
            dv(lambda e: e.tensor_scalar(out=tq[:], in0=CNT[:], scalar1=float(BS - 1), scalar2=1.0 / BS, op0=ALU.add, op1=ALU.mult), ['CNT'], ['tq'])
            dv(lambda e: e.tensor_copy(out=NBI[:], in_=tq[:]), ['tq'], ['NBI'])
            dv(lambda e: e.tensor_copy(out=NBK[:], in_=NBI[:]), ['NBI'], ['NBK'])
            dv(lambda e: e.tensor_tensor(out=CS[:], in0=NBK[:], in1=tq[:], op=ALU.is_gt), ['NBK', 'tq'], ['CS'])
            dv(lambda e: e.tensor_tensor(out=NBK[:], in0=NBK[:], in1=CS[:], op=ALU.subtract), ['NBK', 'CS'], ['NBK'])
            dv(lambda e: e.memset(one32[:], 1.0), [], ['one32'])
            dv(lambda e: e.tensor_tensor_scan(out=CS[:], data0=one32[:], data1=NBK[:], initial=0.0, op0=ALU.mult, op1=ALU.add), ['one32', 'NBK'], ['CS'])
            dv(lambda e: e.tensor_scalar(out=PEND[:], in0=CS[:], scalar1=float(BS), scalar2=None, op0=ALU.mult), ['CS'], ['PEND'])
            dv(lambda e: e.tensor_tensor(out=PST[:], in0=CS[:], in1=NBK[:], op=ALU.subtract), ['CS', 'NBK'], ['PST'])
            dv(lambda e: e.tensor_scalar(out=PST[:], in0=PST[:], scalar1=float(BS), scalar2=None, op0=ALU.mult), ['PST'], ['PST'])
            dv(lambda e: e.tensor_tensor(out=POS[:], in0=POS[:], in1=PST[:].unsqueeze(1).to_broadcast([128, NT, NE]), op=ALU.add), ['POS', 'PST'], ['POS'])
            dv(lambda e: e.scalar_tensor_tensor(out=SM[:], in0=POS[:], scalar=1.0, in1=MK[:], op0=ALU.add, op1=ALU.mult), ['POS'] + MKEYS, ['SM'])
            dv(lambda e: e.tensor_scalar(out=SM[:], in0=SM[:], scalar1=-1.0, scalar2=None, op0=ALU.add), ['SM'], ['SM'])
            for ti in range(NT):
                dv(lambda e, ti=ti: e.max(out=top8[:], in_=SM[:, ti, :]), ['SM'], ['top8'])
                dv(lambda e, ti=ti: e.tensor_copy(out=SL4[:, ti, :], in_=top8[:, 0:4]), ['top8'], ['SL4'])
                dv(lambda e, ti=ti: e.tensor_tensor(out=eq4[:], in0=SM[:, ti, :].unsqueeze(1).to_broadcast([128, 4, NE]),
                                                    in1=top8[:, 0:4].unsqueeze(2).to_broadcast([128, 4, NE]), op=ALU.is_equal), ['SM', 'top8'], ['eq4'])
                dv(lambda e, ti=ti: e.tensor_tensor(out=eq4[:], in0=eq4[:], in1=G[:, ti, :].unsqueeze(1).to_broadcast([128, 4, NE]), op=ALU.mult),
                   ['eq4', ('G', ti)], ['eq4'])
                dv(lambda e, ti=ti: e.tensor_reduce(out=GK[:, ti, :], in_=eq4[:], op=ALU.add, axis=X), ['eq4'], ['GK'])
            dv(lambda e: e.tensor_copy(out=SLI[:], in_=SL4[:]), ['SL4'], ['SLI'])
            P.op('pool', lambda e: e.iota(BTHI[:], pattern=[[BS, NB], [0, NE]], base=0, channel_multiplier=0), w=['BTHI'])
            P.op('pool', lambda e: e.iota(KCI[:], pattern=[[0, NB]], base=0, channel_multiplier=1), w=['KCI'])
            P.op('pool', lambda e: e.iota(PI[:], pattern=[[0, NB]], base=0, channel_multiplier=NE), w=['PI'])
            dv(lambda e: e.tensor_copy(out=BTH[:], in_=BTHI[:]), ['BTHI'], ['BTH'])
            dv(lambda e: e.tensor_copy(out=KCF[:], in_=KCI[:]), ['KCI'], ['KCF'])
            dv(lambda e: e.tensor_copy(out=PF[:], in_=PI[:]), ['PI'], ['PF'])
            dv(lambda e: e.tensor_tensor(out=BTH[:], in0=PEND[:].unsqueeze(1).to_broadcast([128, NB, NE]), in1=BTH[:], op=ALU.is_le), ['PEND', 'BTH'], ['BTH'])
            dv(lambda e: e.tensor_reduce(out=EB[:], in_=BTH[:], op=ALU.add, axis=X), ['BTH'], ['EB'])
            dv(lambda e: e.tensor_scalar(out=EB[:], in0=EB[:], scalar1=float(NE - 1), scalar2=None, op0=ALU.min), ['EB'], ['EB'])
            dv(lambda e: e.tensor_tensor(out=PF[:], in0=PF[:], in1=EB[:], op=ALU.add), ['PF', 'EB'], ['PF'])
            dv(lambda e: e.tensor_copy(out=OFFB[:], in_=PF[:]), ['PF'], ['OFFB'])
            dv(lambda e: e.memset(SK[:], 0.0), [], ['SK'])
            dv(lambda e: e.tensor_tensor(out=SK[:, 2:NB], in0=EB[:, 2:NB], in1=EB[:, 0:NB - 2], op=ALU.is_equal), ['EB', 'SK'], ['SK'])
            dv(lambda e: e.tensor_scalar(out=SK[:], in0=SK[:], scalar1=float(1 << 22), scalar2=None, op0=ALU.mult), ['SK'], ['SK'])
            dv(lambda e: e.tensor_scalar(out=EB[:], in0=EB[:], scalar1=128.0, scalar2=None, op0=ALU.mult), ['EB'], ['EB'])
            dv(lambda e: e.tensor_tensor(out=EB[:], in0=EB[:], in1=SK[:], op=ALU.add), ['EB', 'SK'], ['EB'])
            dv(lambda e: e.tensor_tensor(out=KCF[:], in0=KCF[:], in1=EB[:], op=ALU.add), ['KCF', 'EB'], ['KCF'])
            dv(lambda e: e.tensor_copy(out=OFFW[:], in_=KCF[:]), ['KCF'], ['OFFW'])
            XSZ = []
            for ti in range(NT):
                for k in range(4):
                    P.dma('pool', lambda e, ti=ti, k=k: e.indirect_dma_start(out=xs_d[:, :], out_offset=IOA(ap=SLI[:, ti, k:k + 1], axis=0),
                                                                             in_=HR[:, ti, :], in_offset=None),
                          r=['SLI', ('HR', ti)] + (XSZ if (ti == 0 and k == 0) else []), w=[('xsw', ti, k)])
            P.barrier()
            P.emit(block)

        with ExitStack() as c2:
            WG = [sb(f"WG{i}", [128, 8, 2 * D], BF16, stack=c2) for i in range(2)]
            WD = [sb(f"WD{i}", [128, 8, D], BF16, stack=c2) for i in range(2)]
            BGB = [sb(f"BGB{i}", [128, 16], stack=c2) for i in range(2)]
            XR = [sb(f"XR{i}", [128, 2, D], BF16, stack=c2) for i in range(2)]
            XT = [sb(f"XT{i}", [128, 8, BS], BF16, stack=c2) for i in range(2)]
            ACTT = [sb(f"ACTT{i}", [128, 8, BS], BF16, stack=c2) for i in range(2)]
            YR = [sb(f"YR{i}", [128, D], stack=c2) for i in range(2)]
            gcs = [sb(f"gc{i}", [128, BS], stack=c2) for i in range(2)]
            sgs = [sb(f"sg{i}", [128, BS], stack=c2) for i in range(2)]
            u1s = [sb(f"u1{i}", [128, BS], stack=c2) for i in range(2)]
            ptbs = [pbanks[0][:].bitcast(BF16), pbanks[1][:].bitcast(BF16)]
            pgu = [pbanks[2], pbanks[3]]
            py_ = [pbanks[4], pbanks[5], pbanks[6], pbanks[7]]
            xs_b = xs_d.rearrange("(b s p) d -> b p s d", s=2, p=128)

            BREG = {}

            def _setreg(e):
                BREG['r'] = e.alloc_register('bnd_reg')
                return e.reg_mov(BREG['r'], NE * 128 - 1)
            P.q['pool'].append((_setreg, [], None))
            P.wait_group(['pool'])

            def load_gu(b):
                wb = b % 2
                P.dma('pool', lambda e, wb=wb, b=b: e.indirect_dma_start(out=WG[wb][:].rearrange("p k n -> p (k n)"), out_offset=None,
                                                                         in_=wgu_rows.rearrange("r k n -> r (k n)"),
                                                                         in_offset=IOA(ap=OFFW[:, b:b + 1], axis=0),
                                                                         bounds_check=BREG['r'], oob_is_err=False),
                      w=[('WG', wb, kc) for kc in range(8)])
                P.dma('pool', lambda e, wb=wb, b=b: e.indirect_dma_start(out=BGB[wb][:, :], out_offset=None, in_=bgu_rows[:, :],
                                                                         in_offset=IOA(ap=OFFB[:, b:b + 1], axis=0)), w=[('BGB', wb)])
                P.dma('sp', lambda e, wb=wb, b=b: e.dma_start(out=XR[wb][:], in_=xs_b[b]), w=[('XR', wb)])

            def load_down(b):
                wb = b % 2
                P.dma('pool', lambda e, wb=wb, b=b: e.indirect_dma_start(out=WD[wb][:].rearrange("p k n -> p (k n)"), out_offset=None,
                                                                         in_=wdn_rows.rearrange("r k n -> r (k n)"),
                                                                         in_offset=IOA(ap=OFFW[:, b:b + 1], axis=0),
                                                                         bounds_check=BREG['r'], oob_is_err=False),
                      w=[('WD', wb, fc) for fc in range(8)])

            n_blk = NB if n_exp >= NE else max(2, 2 * n_exp)
            cnts = {'jt': 0, 'yc': 0}

            def do_transposes(b):
                wb = b % 2
                for s_ in range(2):
                    ptb = ptbs[s_]
                    for kc in range(8):
                        P.op('pe', lambda e, s_=s_, kc=kc, wb=wb, ptb=ptb: e.transpose(out=ptb[:, kc * 128:(kc + 1) * 128], in_=XR[wb][:, s_, kc * 128:(kc + 1) * 128], identity=identb[:]),
                             r=[('XR', wb), 'identb'], w=[f'pb{s_}'], sig=(kc == 7))
                    P.op('act', lambda e, s_=s_, wb=wb, ptb=ptb: e.activation(out=XT[wb][:, :, s_ * 128:(s_ + 1) * 128], in_=ptb[:, :].rearrange("p (a b) -> p a b", b=128), func=AF.Identity),
                         r=[f'pb{s_}'], w=[('XT', wb, s_)])

            def do_gu(b):
                wb = b % 2
                for j in range(8):
                    pi = cnts['jt'] % 2
                    cnts['jt'] += 1
                    bank = pgu[pi]
                    for kc in range(8):
                        P.op('pe', lambda e, kc=kc, j=j, wb=wb, bank=bank: e.matmul(bank[:, 0:BS], WG[wb][:, kc, j * 128:(j + 1) * 128], XT[wb][:, kc, :], start=(kc == 0), stop=(kc == 7)),
                             r=[('WG', wb, kc), ('XT', wb, 0), ('XT', wb, 1)], w=[f'pb{2 + pi}'], sig=False)
                    for kc in range(8):
                        P.op('pe', lambda e, kc=kc, j=j, wb=wb, bank=bank: e.matmul(bank[:, BS:2 * BS], WG[wb][:, kc, D + j * 128:D + (j + 1) * 128], XT[wb][:, kc, :], start=(kc == 0), stop=(kc == 7)),
                             r=[('WG', wb, kc), ('XT', wb, 0), ('XT', wb, 1)], w=[f'pb{2 + pi}'], sig=(kc == 7))
                    gc, sg, u1 = gcs[pi], sgs[pi], u1s[pi]
                    P.op('dve', lambda e, gc=gc, bank=bank, j=j, wb=wb: e.tensor_scalar(out=gc[:], in0=bank[:, 0:BS], scalar1=BGB[wb][:, j:j + 1], scalar2=7.0, op0=ALU.add, op1=ALU.min),
                         r=[f'pb{2 + pi}', ('BGB', wb)], w=[f'gc{pi}'])
                    P.op('act', lambda e, gc=gc, sg=sg: e.activation(out=sg[:], in_=gc[:], func=AF.Sigmoid, scale=1.702), r=[f'gc{pi}'], w=[f'sg{pi}'])
                    P.op('dve', lambda e, u1=u1, bank=bank, j=j, wb=wb: e.tensor_scalar(out=u1[:], in0=bank[:, BS:2 * BS], scalar1=BGB[wb][:, 8 + j:9 + j], scalar2=7.0, op0=ALU.add, op1=ALU.min),
                         r=[f'pb{2 + pi}', ('BGB', wb)], w=[f'u1{pi}'])
                    P.op('dve', lambda e, u1=u1: e.tensor_scalar(out=u1[:], in0=u1[:], scalar1=-7.0, scalar2=1.0, op0=ALU.max, op1=ALU.add), r=[f'u1{pi}'], w=[f'u1{pi}'])
                    P.op('pool', lambda e, gc=gc, sg=sg: e.tensor_tensor(out=sg[:], in0=gc[:], in1=sg[:], op=ALU.mult), r=[f'gc{pi}', f'sg{pi}'], w=[f'sg{pi}'])
                    P.op('pool', lambda e, u1=u1, sg=sg, j=j, wb=wb: e.tensor_tensor(out=ACTT[wb][:, j, :], in0=u1[:], in1=sg[:], op=ALU.mult), r=[f'u1{pi}', f'sg{pi}'], w=[('ACTT', wb, j)])

            def do_down(b):
                wb = b % 2
                for s_ in range(2):
                    yr = YR[s_]
                    for half in range(2):
                        yi = cnts['yc'] % 4
                        cnts['yc'] += 1
                        for fc in range(8):
                            P.op('pe', lambda e, fc=fc, s_=s_, half=half, yi=yi, wb=wb: e.matmul(py_[yi][:], ACTT[wb][:, fc, s_ * 128:(s_ + 1) * 128], WD[wb][:, fc, half * 512:(half + 1) * 512],
                                                                                                 start=(fc == 0), stop=(fc == 7)),
                                 r=[('ACTT', wb, fc), ('WD', wb, fc)], w=[f'pb{4 + yi}'], sig=(fc == 7))
                        P.op('act', lambda e, yr=yr, half=half, yi=yi: e.activation(out=yr[:, half * 512:(half + 1) * 512], in_=py_[yi][:], func=AF.Identity),
                             r=[f'pb{4 + yi}'], w=[f'YR{s_}'])
                    r0 = b * BS + s_ * 128
                    P.dma('sp', lambda e, yr=yr, r0=r0: e.dma_start(out=ys_d[r0:r0 + 128, :], in_=yr[:]), r=[f'YR{s_}'], w=[('ys', b, s_)])

            load_gu(0)
            load_down(0)
            if n_blk > 1:
                load_gu(1)
            do_transposes(0)
            for b in range(n_blk):
                do_gu(b)
                if b + 1 < n_blk:
                    do_transposes(b + 1)
                if b >= 1:
                    do_down(b - 1)
                if b + 1 < n_blk:
                    load_down(b + 1)
                if b + 2 < n_blk:
                    load_gu(b + 2)
            do_down(n_blk - 1)
            P.barrier()
            P.emit(block)

        with ExitStack() as c3:
            hts = [sb(f"ht{i}", [128, D], stack=c3) for i in range(2)]
            Y4 = [[sb(f"Y4_{i}_{k}", [128, D], stack=c3) for k in range(4)] for i in range(2)]
            accs = [sb(f"accf{i}", [128, D], stack=c3) for i in range(2)]
            sqj = sb("sqj", [128, D], stack=c3)
            GTt = sb("GTt", [NE, 128], stack=c3)
            ssq = sb("pn_ss", [128, 1], stack=c3)
            rr = sb("pn_rr", [128, 1], stack=c3)
            pgt = pbanks[0]
            pinit = [pbanks[1], pbanks[2]]
            for ti in range(NT):
                k2 = ti % 2
                ht = hts[k2]; acc = accs[k2]
                P.dma('sp', lambda e, ti=ti, ht=ht: e.dma_start(out=ht[:], in_=y_d[ti * 128:(ti + 1) * 128, :]), w=[f'ht{k2}'])
                for k in range(4):
                    P.dma('pool', lambda e, ti=ti, k=k, k2=k2: e.indirect_dma_start(out=Y4[k2][k][:, :], out_offset=None, in_=ys_d[:, :],
                                                                                     in_offset=IOA(ap=SLI[:, ti, k:k + 1], axis=0)), w=[('Y4', k2, k)])
                P.op('pe', lambda e, ti=ti: e.transpose(out=pgt[0:NE, 0:128], in_=G[:, ti, :], identity=ident[:]), w=['pb0'])
                P.op('act', lambda e: e.activation(out=GTt[:], in_=pgt[0:NE, 0:128], func=AF.Identity), r=['pb0'], w=['GTt'])
                for half in range(2):
                    P.op('pe', lambda e, half=half: e.matmul(pinit[half][:], GTt[:], BD[:, half * 512:(half + 1) * 512], start=True, stop=True),
                         r=['GTt'], w=[f'pb{1 + half}'])
                    hs = slice(half * 512, (half + 1) * 512)
                    P.op('dve', lambda e, half=half, hs=hs, acc=acc, ti=ti, k2=k2: e.scalar_tensor_tensor(out=acc[:, hs], in0=Y4[k2][0][:, hs], scalar=GK[:, ti, 0:1], in1=pinit[half][:],
                                                                                                       op0=ALU.mult, op1=ALU.add),
                         r=[f'pb{1 + half}', ('Y4', k2, 0)], w=[f'accf{k2}'])
                for k in range(1, 4):
                    P.op('dve', lambda e, k=k, acc=acc, ti=ti, k2=k2: e.scalar_tensor_tensor(out=acc[:], in0=Y4[k2][k][:], scalar=GK[:, ti, k:k + 1], in1=acc[:], op0=ALU.mult, op1=ALU.add),
                         r=[f'accf{k2}', ('Y4', k2, k)], w=[f'accf{k2}'])
                if debug:
                    P.dma('sp', lambda e, ti=ti, acc=acc: e.dma_start(out=dbg["dbg_acc"][:, ti * D:(ti + 1) * D], in_=acc[:]), r=[f'accf{k2}'])
                P.op('act', lambda e, acc=acc: e.activation(out=sqj[:], in_=acc[:], func=AF.Square, accum_out=ssq[:]), r=[f'accf{k2}'], w=['sqj', 'pn_ss'])
                P.op('act', lambda e: e.activation(out=rr[:], in_=ssq[:], func=AF.Ln, scale=1.0 / D, bias=EPS), r=['pn_ss'], w=['pn_rr'])
                P.op('act', lambda e: e.activation(out=rr[:], in_=rr[:], func=AF.Exp, scale=-0.5), r=['pn_rr'], w=['pn_rr'])
                P.op('dve', lambda e, acc=acc: e.scalar_tensor_tensor(out=acc[:], in0=acc[:], scalar=rr[:, 0:1], in1=GP2[:], op0=ALU.mult, op1=ALU.mult),
                     r=['pn_rr', f'accf{k2}'], w=[f'accf{k2}'])
                P.op('dve', lambda e, acc=acc, ht=ht: e.tensor_tensor(out=acc[:], in0=acc[:], in1=ht[:], op=ALU.add), r=[f'accf{k2}', f'ht{k2}'], w=[f'accf{k2}'])
                P.dma('sp', lambda e, ti=ti, acc=acc: e.dma_start(out=y_d[ti * 128:(ti + 1) * 128, :], in_=acc[:]), r=[f'accf{k2}', f'ht{k2}'], w=[('y', ti)])
            P.barrier()
            P.emit(block)


def build_mixer(nc, P, st, block, L, debug):
    sb = L['sb']; pbanks = L['pbanks']; ident = L['ident']; identb = L['identb']; ones_b = L['ones_b']
    S1, B1, GP1 = L['S1'], L['B1'], L['GP1']
    x_d, y_d = L['x_d'], L['y_d']
    dbg = L['dbg']; dbg_dump = L['dbg_dump']; prenorm_tile = L['prenorm_tile']
    win_d, walpha_d, balpha_d, glag_d = L['win_d'], L['walpha_d'], L['balpha_d'], L['glag_d']
    lre_d, lim_d, ldt_d = L['lre_d'], L['lim_d'], L['ldt_d']
    bre_d, bim_d, cre_d, cim_d, s5d_d = L['bre_d'], L['bim_d'], L['cre_d'], L['cim_d'], L['s5d_d']
    gluw_d, glub_d, wout_d = L['gluw_d'], L['glub_d'], L['wout_d']
    PB_ = ['pb%d' % i for i in range(8)]
    X = mybir.AxisListType.X
    issue_cast_pass = L['issue_cast_pass']

    with ExitStack() as pm:
        MIXIN = sb("MIXIN", [128, 8, T], BF16, stack=pm)
        UT = sb("UT", [128, 4, T], BF16, stack=pm)

        with ExitStack() as m1:
            HN = sb("HN", [128, 8, T], BF16, stack=m1)
            WIN = sb("WIN", [128, 8, 2064], BF16, stack=m1)
            ALR = sb("ALR", [16, T], BF16, stack=m1)
            WA = sb("WA", [16, 256], BF16, stack=m1)
            nba = sb("nba", [64, 4], stack=m1)
            glag = sb("glag", [128, 1], stack=m1)
            xts = [sb(f"xt{i}", [128, D], stack=m1) for i in range(2)]
            ssq2 = [sb(f"pn_ss{i}", [128, 1], stack=m1) for i in range(2)]
            rr2 = [sb(f"pn_rr{i}", [128, 1], stack=m1) for i in range(2)]
            xs2 = [sb(f"pn_xs{i}", [128, D], stack=m1) for i in range(2)]
            for kc in range(8):
                for (c0, c1) in ((0, 1032), (1032, 2064)):
                    P.dma('pool', lambda e, kc=kc, c0=c0, c1=c1: e.dma_start(out=WIN[:, kc, c0:c1], in_=win_d[kc * 128:(kc + 1) * 128, c0:c1]),
                          w=[('WIN', kc, int(c0 > 0))])
            P.dma('pool', lambda e: e.dma_start(out=WA[:], in_=walpha_d[:, :]), w=['WA'])
            P.dma('sp', lambda e: e.dma_start(out=nba[:], in_=balpha_d[:, :]), w=['nba'])
            P.dma('sp', lambda e: e.dma_start(out=glag[:], in_=glag_d[:, :]), w=['glag'])
            P.op('dve', lambda e: e.tensor_scalar(out=nba[:], in0=nba[:], scalar1=-1.0, scalar2=None, op0=ALU.mult), r=['nba'], w=['nba'])
            pst = [pbanks[0], pbanks[1]]
            for ti in range(NT):
                xt = xts[ti % 2]
                P.dma('sp', lambda e, ti=ti, xt=xt: e.dma_start(out=xt[:], in_=x_d[ti * 128:(ti + 1) * 128, :]), w=[f'xt{ti % 2}'])
                prenorm_tile(xt[:], [f'xt{ti % 2}'], ti, HN, lambda kc, ti_: ('HN', kc, ti_ // 4), S1, B1, (ssq2[ti % 2], rr2[ti % 2], xs2[ti % 2]), pst, sx=str(ti % 2))
            if debug:
                pass
            WINK = [('WIN', kc) for kc in range(8)]

            pj = [0]

            def proj(cols, M, tg_or_n, token_major=False):
                bi = pj[0] % 2
                pj[0] += 1
                bank = pbanks[bi]
                key = PB_[bi]
                return bank, key

            for tg in range(4):
                bank, key = proj(None, None, None)
                for kc in range(8):
                    P.op('pe', lambda e, kc=kc, tg=tg, bank=bank: e.matmul(bank[0:16, :], WIN[:, kc, 1536:1552], HN[:, kc, tg * 512:(tg + 1) * 512],
                                                                           start=(kc == 0), stop=(kc == 7)),
                         r=[('WIN', kc, 0), ('WIN', kc, 1), ('HN', kc, tg)], w=[key], sig=(kc == 7))
                P.op('act', lambda e, tg=tg, bank=bank: e.activation(out=ALR[:, tg * 512:(tg + 1) * 512], in_=bank[0:16, :], func=AF.Identity),
                     r=[key], w=[('ALR', tg)])
            for cc in range(4):
                for tg in range(4):
                    bank, key = proj(None, None, None)
                    for kc in range(8):
                        P.op('pe', lambda e, kc=kc, tg=tg, cc=cc, bank=bank: e.matmul(
                            bank[:], WIN[:, kc, 1552 + cc * 128:1552 + (cc + 1) * 128], HN[:, kc, tg * 512:(tg + 1) * 512], start=(kc == 0), stop=(kc == 7)),
                            r=[('WIN', kc, 0), ('WIN', kc, 1), ('HN', kc, tg)], w=[key], sig=(kc == 7))
                    P.op('act', lambda e, tg=tg, cc=cc, bank=bank: e.activation(out=UT[:, cc, tg * 512:(tg + 1) * 512], in_=bank[:], func=AF.Identity),
                         r=[key], w=[('UT', cc, tg)])

            with ExitStack() as g:
                T1 = sb("T1", [64, T], stack=g)
                T2 = sb("T2", [64, T], stack=g)
                QD = sb("QD", [64, T], BF16, stack=g)
                KI = sb("KI", [64, T], BF16, stack=g)
                KE = sb("KE", [64, T], BF16, stack=g)
                KEt = sb("KEt", [64, 32, 64], BF16, stack=g)
                V = sb("V", [64, 32, 128], BF16, stack=g)
                SOG = sb("SOG", [128, T], BF16, stack=g)
                Sf = [sb(f"Sf{i}", [64, 128], stack=g) for i in range(2)]
                Sb = sb("Sb", [64, 32, 128], BF16, stack=g)
                mask01 = sb("mask01", [64, T], BF16, stack=g)
                cmask = sb("cmask", [64, 64], stack=g)
                scTm = [sb(f"scTm{i}", [64, 8, 64], BF16, stack=g) for i in range(2)]
                sq = sb("sq", [128, 512], BF16, stack=g)
                rn = sb("rn", [128, 512], stack=g)
                tt_ = sb("tt_", [128, 512], stack=g)
                P.op('pool', lambda e: e.memset(mask01[:], 1.0), w=['mask01'])
                P.op('pool', lambda e: e.memset(mask01[:, 0::64], 0.0), w=['mask01'])
                P.op('pool', lambda e: e.memset(cmask[:], 1.0), w=['cmask'])
                P.op('pool', lambda e: e.affine_select(out=cmask[:], in_=cmask[:], pattern=[[1, 64]], compare_op=ALU.is_ge,
                                                       fill=0.0, base=0, channel_multiplier=-1), r=['cmask'], w=['cmask'])
                T1v = T1[:].rearrange("p (n t) -> p n t", t=64)
                issue_cast_pass(0, NE)
                for h in range(4):
                    for tg in range(4):
                        bank, key = proj(None, None, None)
                        P.op('pe', lambda e, tg=tg, h=h, bank=bank: e.matmul(bank[0:64, :], WA[:, h * 64:(h + 1) * 64], ALR[:, tg * 512:(tg + 1) * 512],
                                                                             start=True, stop=True),
                             r=['WA', ('ALR', tg)], w=[key])
                        P.op('act', lambda e, tg=tg, h=h, bank=bank: e.activation(out=T1[:, tg * 512:(tg + 1) * 512], in_=bank[0:64, :], func=AF.Exp,
                                                                                  scale=-1.0, bias=nba[:, h:h + 1]),
                             r=[key, 'nba'], w=['T1'])
                    P.op('act', lambda e: e.activation(out=T1[:], in_=T1[:], func=AF.Ln, bias=1.0, scale=1.0), r=['T1'], w=['T1'])
                    P.op('dve', lambda e: e.tensor_tensor_scan(out=T2[:], data0=mask01[:], data1=T1[:], initial=0.0, op0=ALU.mult, op1=ALU.add),
                         r=['T1', 'mask01'], w=['T2'])
                    P.op('act', lambda e: e.activation(out=T1[:], in_=T2[:], func=AF.Exp, scale=-1.0 / 16.0), r=['T2'], w=['T1'])
                    P.op('act', lambda e: e.activation(out=T2[:], in_=T2[:], func=AF.Exp, scale=1.0 / 16.0), r=['T2'], w=['T2'])
                    for tg in range(4):
                        bank, key = proj(None, None, None)
                        for kc in range(8):
                            P.op('pe', lambda e, kc=kc, tg=tg, h=h, bank=bank: e.matmul(bank[0:64, :], WIN[:, kc, h * 64:(h + 1) * 64], HN[:, kc, tg * 512:(tg + 1) * 512],
                                                                                        start=(kc == 0), stop=(kc == 7)),
                                 r=[('WIN', kc, 0), ('WIN', kc, 1), ('HN', kc, tg)], w=[key], sig=(kc == 7))
                        P.op('dve', lambda e, tg=tg, bank=bank: e.scalar_tensor_tensor(out=QD[:, tg * 512:(tg + 1) * 512], in0=bank[0:64, :], scalar=0.125,
                                                                                       in1=T1[:, tg * 512:(tg + 1) * 512], op0=ALU.mult, op1=ALU.mult),
                             r=[key, 'T1'], w=[('QD', tg)])
                    for tg in range(4):
                        bank, key = proj(None, None, None)
                        for kc in range(8):
                            P.op('pe', lambda e, kc=kc, tg=tg, h=h, bank=bank: e.matmul(bank[0:64, :], WIN[:, kc, 256 + h * 64:256 + (h + 1) * 64],
                                                                                        HN[:, kc, tg * 512:(tg + 1) * 512], start=(kc == 0), stop=(kc == 7)),
                                 r=[('WIN', kc, 0), ('WIN', kc, 1), ('HN', kc, tg)], w=[key], sig=(kc == 7))
                        P.op('dve', lambda e, tg=tg, bank=bank: e.tensor_tensor(out=KI[:, tg * 512:(tg + 1) * 512], in0=bank[0:64, :],
                                                                                in1=T2[:, tg * 512:(tg + 1) * 512], op=ALU.mult),
                             r=[key, 'T2'], w=[('KI', tg)])
                    P.op('dve', lambda e: e.tensor_tensor(out=KE[:].rearrange("p (n t) -> p n t", t=64), in0=KI[:].rearrange("p (n t) -> p n t", t=64),
                                                           in1=T1v[:, :, 63:64].to_broadcast([64, 32, 64]), op=ALU.mult),
                         r=[('KI', tg) for tg in range(4)] + ['T1'], w=['KE'])
                    ptb = pbanks[3][:].bitcast(BF16)
                    for n8 in range(4):
                        for q in range(8):
                            n = n8 * 8 + q
                            P.op('pe', lambda e, n=n, q=q: e.transpose(out=ptb[0:64, q * 64:(q + 1) * 64], in_=KE[:, n * 64:(n + 1) * 64], identity=identb[0:64, 0:64]),
                                 r=['KE', 'identb'], w=[PB_[3]], sig=(q == 7))
                        P.op('act', lambda e, n8=n8: e.activation(out=KEt[:, n8 * 8:(n8 + 1) * 8, :], in_=ptb[0:64, 0:512].rearrange("p (a b) -> p a b", b=64), func=AF.Identity),
                             r=[PB_[3]], w=[('KEt', n8)])
                    for n4 in range(8):
                        bank, key = proj(None, None, None)
                        for q in range(4):
                            n = n4 * 4 + q
                            for kc in range(8):
                                P.op('pe', lambda e, kc=kc, n=n, q=q, h=h, bank=bank: e.matmul(
                                    bank[0:64, q * 128:(q + 1) * 128], HN[:, kc, n * 64:(n + 1) * 64], WIN[:, kc, 512 + h * 128:512 + (h + 1) * 128],
                                    start=(kc == 0), stop=(kc == 7)),
                                    r=[('WIN', kc, 0), ('WIN', kc, 1), ('HN', kc, n // 8)], w=[key], sig=(kc == 7 and q == 3))
                        P.op('act', lambda e, n4=n4, bank=bank: e.activation(out=V[:, n4 * 4:(n4 + 1) * 4, :], in_=bank[0:64, :].rearrange("p (a b) -> p a b", b=128), func=AF.Identity),
                             r=[key], w=[('V', n4)])
                    for tg in range(4):
                        bank, key = proj(None, None, None)
                        for kc in range(8):
                            P.op('pe', lambda e, kc=kc, tg=tg, h=h, bank=bank: e.matmul(bank[:], WIN[:, kc, 1024 + h * 128:1024 + (h + 1) * 128],
                                                                                        HN[:, kc, tg * 512:(tg + 1) * 512], start=(kc == 0), stop=(kc == 7)),
                                 r=[('WIN', kc, 0), ('WIN', kc, 1), ('HN', kc, tg)], w=[key], sig=(kc == 7))
                        P.op('act', lambda e, tg=tg, bank=bank: e.activation(out=SOG[:, tg * 512:(tg + 1) * 512], in_=bank[:], func=AF.Silu), r=[key], w=[('SOG', tg)])
                    for n8 in range(4):
                        bi = 2 + (n8 % 2)
                        bank = pbanks[bi]
                        for q in range(8):
                            n = n8 * 8 + q
                            P.op('pe', lambda e, n=n, q=q, bank=bank: e.matmul(bank[0:64, q * 64:(q + 1) * 64], KI[:, n * 64:(n + 1) * 64], QD[:, n * 64:(n + 1) * 64],
                                                                               start=True, stop=True),
                                 r=[('KI', n // 8), ('QD', n // 8)], w=[PB_[bi]], sig=(q == 7))
                        P.op('dve', lambda e, n8=n8, bank=bank: e.tensor_tensor(out=scTm[n8 % 2][:], in0=bank[0:64, :].rearrange("p (a b) -> p a b", b=64),
                                                                                in1=cmask[:].unsqueeze(1).to_broadcast([64, 8, 64]), op=ALU.mult),
                             r=[PB_[bi], 'cmask'], w=[('scTm', n8 % 2)])
                        if n8 == 0:
                            P.op('dve', lambda e: e.memset(Sf[0][:], 0.0), w=['Sf0'])
                            P.op('dve', lambda e: e.memset(Sb[:, 0, :], 0.0), w=[('Sb', 0)])
                            for n4 in range(8):
                                dbi = 4 + (n4 % 2)
                                dbank = pbanks[dbi]
                                for q in range(4):
                                    n = n4 * 4 + q
                                    if n == 31:
                                        continue
                                    P.op('pe', lambda e, n=n, q=q, dbank=dbank: e.matmul(dbank[0:64, q * 128:(q + 1) * 128], KEt[:, n, :], V[:, n, :], start=True, stop=True),
                                         r=[('KEt', n // 8), ('V', n // 4)], w=[PB_[dbi]], sig=(q == 3 or n == 30))
                                for q in range(4):
                                    n = n4 * 4 + q
                                    if n == 31:
                                        continue
                                    so, sn = Sf[n % 2], Sf[(n + 1) % 2]
                                    P.op('dve', lambda e, n=n, q=q, so=so, sn=sn, dbank=dbank: e.scalar_tensor_tensor(
                                        out=sn[:], in0=so[:], scalar=T1v[:, n, 63:64], in1=dbank[0:64, q * 128:(q + 1) * 128], op0=ALU.mult, op1=ALU.add),
                                        r=[f'Sf{n % 2}', 'T1', PB_[dbi]], w=[f'Sf{(n + 1) % 2}'])
                                    P.op('act', lambda e, n=n, sn=sn: e.activation(out=Sb[:, n + 1, :], in_=sn[:], func=AF.Identity),
                                         r=[f'Sf{(n + 1) % 2}'], w=[('Sb', n + 1)])
                        tg = n8
                        po = pbanks[6]
                        for q in range(8):
                            n = n8 * 8 + q
                            P.op('pe', lambda e, n=n, q=q, n8=n8: e.matmul(po[:, q * 64:(q + 1) * 64], V[:, n, :], scTm[n8 % 2][:, q, :], start=True, stop=False),
                                 r=[('V', n // 4), ('scTm', n8 % 2)], w=[PB_[6]], sig=False)
                            P.op('pe', lambda e, n=n, q=q: e.matmul(po[:, q * 64:(q + 1) * 64], Sb[:, n, :], QD[:, n * 64:(n + 1) * 64], start=False, stop=True),
                                 r=[('Sb', n), ('QD', n // 8)], w=[PB_[6]], sig=(q == 7))
                        P.op('act', lambda e: e.activation(out=sq[:], in_=po[:], func=AF.Square), r=[PB_[6]], w=['sq'])
                        pss = pbanks[7]
                        P.op('pe', lambda e: e.matmul(pss[:], ones_b[:], sq[:], start=True, stop=True), r=['sq', 'ones_b'], w=[PB_[7]])
                        P.op('act', lambda e: e.activation(out=rn[:], in_=pss[:], func=AF.Ln, scale=1.0 / 128.0, bias=EPS), r=[PB_[7]], w=['rn'])
                        P.op('act', lambda e: e.activation(out=rn[:], in_=rn[:], func=AF.Exp, scale=-0.5), r=['rn'], w=['rn'])
                        P.op('dve', lambda e: e.scalar_tensor_tensor(out=tt_[:], in0=po[:], scalar=glag[:, 0:1], in1=rn[:], op0=ALU.mult, op1=ALU.mult),
                             r=[PB_[6], 'rn', 'glag'], w=['tt_'])
                        P.op('dve', lambda e, tg=tg, h=h: e.tensor_tensor(out=MIXIN[:, h, tg * 512:(tg + 1) * 512], in0=tt_[:], in1=SOG[:, tg * 512:(tg + 1) * 512], op=ALU.mult),
                             r=['tt_', ('SOG', tg)], w=[('MIXIN', h, tg)])
            P.barrier()
            P.emit(block)

        with ExitStack() as m2:
            def t16(name):
                return sb(name, [128, 16], stack=m2)
            lre = t16("lre"); lim = t16("lim"); dtt = t16("dtt"); mag = t16("mag")
            A2 = sb("A2", [128, 32], stack=m2); K2 = sb("K2", [128, 32], stack=m2); KI2 = sb("KI2", [128, 32], I32, stack=m2)
            M2_ = sb("M2_", [128, 32], stack=m2); SN = sb("SN", [128, 32], stack=m2)
            den = t16("den"); am1 = t16("am1"); fre = t16("fre"); fim = t16("fim"); tA = t16("tA"); tB = t16("tB")
            PW = sb("PW", [128, 11, 3, 16], stack=m2)
            bre = sb("bre", [128, 16, 16], stack=m2); bim = sb("bim", [128, 16, 16], stack=m2)
            cre = sb("cre", [128, 16, 16], stack=m2); cim = sb("cim", [128, 16, 16], stack=m2)
            bbr = sb("bbr", [128, 16, 16], stack=m2); bbi = sb("bbi", [128, 16, 16], stack=m2); tb3 = sb("tb3", [128, 16, 16], stack=m2)
            s5d = sb("s5d", [128, 4], stack=m2); glub = sb("glub", [128, 4], stack=m2)
            PBm = sb("PBm", [128, 16, 2, 128], stack=m2)
            WB = sb("WB", [128, 32, 128], BF16, stack=m2)
            WC = sb("WC", [128, 16, 2, 128], BF16, stack=m2)
            GW = sb("GW", [128, 4, 512], BF16, stack=m2)
            GWf = sb("GWf", [128, 512], stack=m2)
            XP = [sb(f"XP{i}", [128, 2, T], stack=m2) for i in range(2)]
            XS = [[XP[i][:, 0, :], XP[i][:, 1, :]] for i in range(2)]
            GT_ = sb("GT_", [128, T], stack=m2)
            XBF = sb("XBF", [128, 4, 2, T], BF16, stack=m2)
            YT = sb("YT", [128, T], stack=m2)
            YG = sb("YG", [128, 4, T], BF16, stack=m2)
            sgl = sb("sgl", [128, 512], stack=m2)
            for nm, t_, d_ in (("lre", lre, lre_d), ("lim", lim, lim_d), ("dtt", dtt, ldt_d), ("s5d", s5d, s5d_d), ("glub", glub, glub_d)):
                P.dma('sp', lambda e, t_=t_, d_=d_: e.dma_start(out=t_[:], in_=d_[:, :]), w=[nm])
            for nm, t_, d_ in (("bre", bre, bre_d), ("bim", bim, bim_d), ("cre", cre, cre_d), ("cim", cim, cim_d)):
                P.dma('sp', lambda e, t_=t_, d_=d_: e.dma_start(out=t_[:], in_=d_[:, :, :]), w=[nm])
            for cc in range(4):
                P.dma('sp', lambda e, cc=cc: e.dma_start(out=GWf[:], in_=gluw_d[cc * 128:(cc + 1) * 128, :]), w=['GWf'])
                P.op('act', lambda e, cc=cc: e.activation(out=GW[:, cc, :], in_=GWf[:], func=AF.Identity), r=['GWf'], w=[('GW', cc)])

            def dv(fn, r, w):
                return P.op('dve', fn, r=r, w=w)
            TWO_PI = 2.0 * math.pi
            P.op('act', lambda e: e.activation(out=dtt[:], in_=dtt[:], func=AF.Exp), r=['dtt'], w=['dtt'])
            dv(lambda e: e.tensor_tensor(out=tA[:], in0=lre[:], in1=dtt[:], op=ALU.mult), ['lre', 'dtt'], ['tA'])
            P.op('act', lambda e: e.activation(out=mag[:], in_=tA[:], func=AF.Exp), r=['tA'], w=['mag'])
            dv(lambda e: e.tensor_tensor(out=A2[:, 0:16], in0=lim[:], in1=dtt[:], op=ALU.mult), ['lim', 'dtt'], ['A2'])
            dv(lambda e: e.tensor_scalar(out=A2[:, 16:32], in0=A2[:, 0:16], scalar1=math.pi / 2.0, scalar2=None, op0=ALU.add), ['A2'], ['A2'])
            dv(lambda e: e.tensor_scalar(out=K2[:], in0=A2[:], scalar1=1.0 / TWO_PI, scalar2=None, op0=ALU.mult), ['A2'], ['K2'])
            dv(lambda e: e.tensor_copy(out=KI2[:], in_=K2[:]), ['K2'], ['KI2'])
            dv(lambda e: e.tensor_copy(out=K2[:], in_=KI2[:]), ['KI2'], ['K2'])
            dv(lambda e: e.scalar_tensor_tensor(out=A2[:], in0=K2[:], scalar=-TWO_PI, in1=A2[:], op0=ALU.mult, op1=ALU.add), ['K2', 'A2'], ['A2'])
            dv(lambda e: e.tensor_scalar(out=M2_[:], in0=A2[:], scalar1=math.pi, scalar2=None, op0=ALU.is_gt), ['A2'], ['M2_'])
            dv(lambda e: e.scalar_tensor_tensor(out=A2[:], in0=M2_[:], scalar=-TWO_PI, in1=A2[:], op0=ALU.mult, op1=ALU.add), ['M2_', 'A2'], ['A2'])
            dv(lambda e: e.tensor_scalar(out=M2_[:], in0=A2[:], scalar1=-math.pi, scalar2=None, op0=ALU.is_lt), ['A2'], ['M2_'])
            dv(lambda e: e.scalar_tensor_tensor(out=A2[:], in0=M2_[:], scalar=TWO_PI, in1=A2[:], op0=ALU.mult, op1=ALU.add), ['M2_', 'A2'], ['A2'])
            dv(lambda e: e.tensor_scalar(out=A2[:], in0=A2[:], scalar1=math.pi, scalar2=-math.pi, op0=ALU.min, op1=ALU.max), ['A2'], ['A2'])
            P.op('act', lambda e: e.activation(out=SN[:], in_=A2[:], func=AF.Sin), r=['A2'], w=['SN'])
            AR0 = PW[:, 0, 0, :]; AI0 = PW[:, 0, 1, :]; NAI0 = PW[:, 0, 2, :]
            dv(lambda e: e.tensor_tensor(out=AR0, in0=mag[:], in1=SN[:, 16:32], op=ALU.mult), ['mag', 'SN'], ['PW'])
            dv(lambda e: e.tensor_tensor(out=AI0, in0=mag[:], in1=SN[:, 0:16], op=ALU.mult), ['mag', 'SN'], ['PW'])
            dv(lambda e: e.tensor_scalar(out=NAI0, in0=AI0, scalar1=-1.0, scalar2=None, op0=ALU.mult), ['PW'], ['PW'])
            for k in range(1, 11):
                a_r = PW[:, k - 1, 0, :]; a_i = PW[:, k - 1, 1, :]
                n_r = PW[:, k, 0, :]; n_i = PW[:, k, 1, :]; n_ni = PW[:, k, 2, :]
                dv(lambda e, a_r=a_r: e.tensor_tensor(out=tA[:], in0=a_r, in1=a_r, op=ALU.mult), ['PW'], ['tA'])
                dv(lambda e, a_i=a_i: e.tensor_tensor(out=tB[:], in0=a_i, in1=a_i, op=ALU.mult), ['PW'], ['tB'])
                dv(lambda e, n_r=n_r: e.tensor_tensor(out=n_r, in0=tA[:], in1=tB[:], op=ALU.subtract), ['tA', 'tB'], ['PW'])
                dv(lambda e, a_r=a_r, a_i=a_i: e.tensor_tensor(out=tA[:], in0=a_r, in1=a_i, op=ALU.mult), ['PW'], ['tA'])
                dv(lambda e, n_i=n_i: e.tensor_scalar(out=n_i, in0=tA[:], scalar1=2.0, scalar2=None, op0=ALU.mult), ['tA'], ['PW'])
                dv(lambda e, n_ni=n_ni: e.tensor_scalar(out=n_ni, in0=tA[:], scalar1=-2.0, scalar2=None, op0=ALU.mult), ['tA'], ['PW'])
            dv(lambda e: e.tensor_tensor(out=den[:], in0=lre[:], in1=lre[:], op=ALU.mult), ['lre'], ['den'])
            dv(lambda e: e.tensor_tensor(out=tA[:], in0=lim[:], in1=lim[:], op=ALU.mult), ['lim'], ['tA'])
            dv(lambda e: e.tensor_tensor(out=den[:], in0=den[:], in1=tA[:], op=ALU.add), ['den', 'tA'], ['den'])
            dv(lambda e: e.reciprocal(out=den[:], in_=den[:]), ['den'], ['den'])
            dv(lambda e: e.tensor_scalar(out=am1[:], in0=AR0, scalar1=-1.0, scalar2=None, op0=ALU.add), ['PW'], ['am1'])
            dv(lambda e: e.tensor_tensor(out=tA[:], in0=am1[:], in1=lre[:], op=ALU.mult), ['am1', 'lre'], ['tA'])
            dv(lambda e: e.tensor_tensor(out=tB[:], in0=AI0, in1=lim[:], op=ALU.mult), ['PW', 'lim'], ['tB'])
            dv(lambda e: e.tensor_tensor(out=tA[:], in0=tA[:], in1=tB[:], op=ALU.add), ['tA', 'tB'], ['tA'])
            dv(lambda e: e.tensor_tensor(out=fre[:], in0=tA[:], in1=den[:], op=ALU.mult), ['tA', 'den'], ['fre'])
            dv(lambda e: e.tensor_tensor(out=tA[:], in0=AI0, in1=lre[:], op=ALU.mult), ['PW', 'lre'], ['tA'])
            dv(lambda e: e.tensor_tensor(out=tB[:], in0=am1[:], in1=lim[:], op=ALU.mult), ['am1', 'lim'], ['tB'])
            dv(lambda e: e.tensor_tensor(out=tA[:], in0=tA[:], in1=tB[:], op=ALU.subtract), ['tA', 'tB'], ['tA'])
            dv(lambda e: e.tensor_tensor(out=fim[:], in0=tA[:], in1=den[:], op=ALU.mult), ['tA', 'den'], ['fim'])
            freb = fre[:].unsqueeze(2).to_broadcast([128, 16, 16]); fimb = fim[:].unsqueeze(2).to_broadcast([128, 16, 16])
            dv(lambda e: e.tensor_tensor(out=bbr[:], in0=bre[:], in1=freb, op=ALU.mult), ['bre', 'fre'], ['bbr'])
            dv(lambda e: e.tensor_tensor(out=tb3[:], in0=bim[:], in1=fimb, op=ALU.mult), ['bim', 'fim'], ['tb3'])
            dv(lambda e: e.tensor_tensor(out=bbr[:], in0=bbr[:], in1=tb3[:], op=ALU.subtract), ['bbr', 'tb3'], ['bbr'])
            dv(lambda e: e.tensor_tensor(out=bbi[:], in0=bim[:], in1=freb, op=ALU.mult), ['bim', 'fre'], ['bbi'])
            dv(lambda e: e.tensor_tensor(out=tb3[:], in0=bre[:], in1=fimb, op=ALU.mult), ['bre', 'fim'], ['tb3'])
            dv(lambda e: e.tensor_tensor(out=bbi[:], in0=bbi[:], in1=tb3[:], op=ALU.add), ['bbi', 'tb3'], ['bbi'])
            P.op('dve', lambda e: e.memset(PBm[:], 0.0), w=['PBm'])
            P.op('dve', lambda e: e.memset(WC[:], 0.0), w=['WC'])
            for j in range(4):
                for ri, (bsrc, bkey, csrc, ckey, csign) in enumerate(((bbr, 'bbr', cre, 'cre', 1.0), (bbi, 'bbi', cim, 'cim', -1.0))):
                    for hf in range(2):
                        prt = slice(hf * 64, (hf + 1) * 64)
                        cs = slice(32 * j + 16 * hf, 32 * j + 16 * hf + 16)
                        dv(lambda e, bsrc=bsrc, prt=prt, cs=cs, ri=ri, j=j: e.tensor_copy(out=PBm[prt, j::4, ri, cs], in_=bsrc[prt, j::4, :]), [bkey, 'PBm'], ['PBm'])
                        dv(lambda e, csrc=csrc, prt=prt, cs=cs, ri=ri, j=j, csign=csign: e.tensor_scalar(
                            out=WC[prt, j::4, ri, cs], in0=csrc[prt, j::4, :], scalar1=csign, scalar2=None, op0=ALU.mult), [ckey, 'WC'], ['WC'])
            PBf = PBm[:].rearrange("p m r c -> p (m r) c")
            for i4 in range(8):
                bi = i4 % 2
                bank = pbanks[bi]
                for q in range(4):
                    idx = i4 * 4 + q
                    P.op('pe', lambda e, idx=idx, q=q, bank=bank: e.transpose(out=bank[:, q * 128:(q + 1) * 128], in_=PBf[:, idx, :], identity=ident[:]),
                         r=['PBm', 'ident'], w=[PB_[bi]], sig=(q == 3))
                P.op('act', lambda e, i4=i4, bank=bank: e.activation(out=WB[:, i4 * 4:(i4 + 1) * 4, :], in_=bank[:].rearrange("p (a b) -> p a b", b=128), func=AF.Identity),
                     r=[PB_[bi]], w=['WB'])
            WCf = WC[:].rearrange("p m r c -> p (m r) c")

            pjc = [0]
            for cc in range(4):
                for jp in range(2):
                    ms = [4 * cc + 2 * jp, 4 * cc + 2 * jp + 1]
                    for m in ms:
                        for ri in range(2):
                            for tg in range(4):
                                bi = pjc[0] % 2
                                pjc[0] += 1
                                bank = pbanks[bi]
                                P.op('pe', lambda e, m=m, ri=ri, tg=tg, cc=cc, bank=bank: e.matmul(bank[:], WB[:, 2 * m + ri, :], UT[:, cc, tg * 512:(tg + 1) * 512], start=True, stop=True),
                                     r=['WB'], w=[PB_[bi]])
                                P.op('act', lambda e, ri=ri, tg=tg, bank=bank, m=m: e.activation(out=XP[m % 2][:, ri, tg * 512:(tg + 1) * 512], in_=bank[:], func=AF.Identity),
                                     r=[PB_[bi]], w=[f'X{m % 2}' + 'ri'[ri]])

                    def bk_level(d, start):
                        step = 1 << (d + 1)
                        hop = 1 << d
                        if start >= T:
                            return
                        cnt = (T - 1 - start) // step + 1
                        w_ = lambda Xt: Xt[:, start:start + (cnt - 1) * step + 1:step]
                        r_ = lambda Xt: Xt[:, start - hop:start - hop + (cnt - 1) * step + 1:step]
                        w2_ = lambda Xp: Xp[:, :, start:start + (cnt - 1) * step + 1:step]
                        r2_ = lambda Xp: Xp[:, :, start - hop:start - hop + (cnt - 1) * step + 1:step]
                        for phase in range(2):
                            for m in ms:
                                Xp = XP[m % 2]
                                Xr, Xi = XS[m % 2]
                                kr, ki = f'X{m % 2}r', f'X{m % 2}i'
                                ARk = PW[:, d, 0, m:m + 1]; AIk = PW[:, d, 1, m:m + 1]; NAIk = PW[:, d, 2, m:m + 1]
                                if phase == 0:
                                    dv(lambda e, Xp=Xp, ARk=ARk: e.scalar_tensor_tensor(out=w2_(Xp), in0=r2_(Xp), scalar=ARk, in1=w2_(Xp), op0=ALU.mult, op1=ALU.add), [kr, ki], [kr, ki])
                                else:
                                    dv(lambda e, Xr=Xr, Xi=Xi, NAIk=NAIk: e.scalar_tensor_tensor(out=w_(Xr), in0=r_(Xi), scalar=NAIk, in1=w_(Xr), op0=ALU.mult, op1=ALU.add), [kr, ki], [kr])
                                    dv(lambda e, Xr=Xr, Xi=Xi, AIk=AIk: e.scalar_tensor_tensor(out=w_(Xi), in0=r_(Xr), scalar=AIk, in1=w_(Xi), op0=ALU.mult, op1=ALU.add), [kr, ki], [ki])
                    for d in range(11):
                        bk_level(d, (1 << (d + 1)) - 1)
                    for d in range(9, -1, -1):
                        bk_level(d, 3 * (1 << d) - 1)
                    for m in ms:
                        jm = m - 4 * cc
                        for ri in range(2):
                            P.op('act', lambda e, ri=ri, jm=jm, Xt=XS[m % 2][ri]: e.activation(out=XBF[:, jm, ri, :], in_=Xt, func=AF.Identity),
                                 r=[f'X{m % 2}' + 'ri'[ri]], w=[('XBF', jm, ri)])
                for tg in range(4):
                    bi = 2 + (tg % 2)
                    bank = pbanks[bi]
                    i_ = 0
                    for jm in range(4):
                        m = 4 * cc + jm
                        for ri in range(2):
                            P.op('pe', lambda e, m=m, ri=ri, jm=jm, tg=tg, bank=bank, i_=i_: e.matmul(bank[:], WCf[:, 2 * m + ri, :], XBF[:, jm, ri, tg * 512:(tg + 1) * 512],
                                                                                                      start=(i_ == 0), stop=(i_ == 7)),
                                 r=['WC', ('XBF', jm, ri)], w=[PB_[bi]], sig=(i_ == 7))
                            i_ += 1
                    dv(lambda e, tg=tg, cc=cc, bank=bank: e.scalar_tensor_tensor(out=YT[:, tg * 512:(tg + 1) * 512], in0=UT[:, cc, tg * 512:(tg + 1) * 512], scalar=s5d[:, cc:cc + 1],
                                                                                 in1=bank[:], op0=ALU.mult, op1=ALU.add),
                       [PB_[bi], 's5d'], [('YT', tg)])
                YTK = [('YT', tg) for tg in range(4)]
                P.op('act', lambda e: e.activation(out=GT_[:], in_=YT[:], func=AF.Square), r=YTK, w=['GT_'])
                P.op('act', lambda e: e.activation(out=GT_[:], in_=GT_[:], func=AF.Identity, scale=0.044715, bias=1.0), r=['GT_'], w=['GT_'])
                P.op('dve', lambda e: e.tensor_tensor(out=GT_[:], in0=GT_[:], in1=YT[:], op=ALU.mult), r=['GT_'] + YTK, w=['GT_'])
                P.op('act', lambda e: e.activation(out=GT_[:], in_=GT_[:], func=AF.Sigmoid, scale=2.0 * math.sqrt(2.0 / math.pi)), r=['GT_'], w=['GT_'])
                P.op('dve', lambda e, cc=cc: e.tensor_tensor(out=YG[:, cc, :], in0=GT_[:], in1=YT[:], op=ALU.mult), r=['GT_'] + YTK, w=[('YG', cc)])
            for oc in range(4):
                for tg in range(4):
                    bi = 4 + (tg % 2)
                    bank = pbanks[bi]
                    for cc in range(4):
                        P.op('pe', lambda e, oc=oc, tg=tg, cc=cc, bank=bank: e.matmul(bank[:], GW[:, cc, oc * 128:(oc + 1) * 128], YG[:, cc, tg * 512:(tg + 1) * 512],
                                                                                      start=(cc == 0), stop=(cc == 3)),
                             r=[('GW', cc), ('YG', cc)], w=[PB_[bi]], sig=(cc == 3))
                    P.op('act', lambda e, oc=oc, bank=bank: e.activation(out=sgl[:], in_=bank[:], func=AF.Sigmoid, bias=glub[:, oc:oc + 1], scale=1.0),
                         r=[PB_[bi], 'glub'], w=['sgl'])
                    dv(lambda e, oc=oc, tg=tg: e.tensor_tensor(out=MIXIN[:, 4 + oc, tg * 512:(tg + 1) * 512], in0=YG[:, oc, tg * 512:(tg + 1) * 512], in1=sgl[:], op=ALU.mult),
                       ['sgl', ('YG', oc)], [('MIXIN', 4 + oc, tg)])
            P.barrier()
            P.emit(block)

        if debug:
            for c in range(8):
                pass

        with ExitStack() as m3:
            WO = sb("WO", [128, 8, D], BF16, stack=m3)
            WOf = [sb(f"WOf{i}", [128, D], stack=m3) for i in range(2)]
            mixt = [sb(f"mixt{i}", [128, D], stack=m3) for i in range(4)]
            xts = [sb(f"xt{i}", [128, D], stack=m3) for i in range(NT)]
            jk = sb("jk", [128, D], stack=m3)
            ssq = sb("pn_ss", [128, 1], stack=m3)
            rr = sb("pn_rr", [128, 1], stack=m3)
            for c in range(8):
                P.dma('sp', lambda e, c=c: e.dma_start(out=WOf[c % 2][:], in_=wout_d[c * 128:(c + 1) * 128, :]), w=[f'WOf{c % 2}'])
                P.op('act', lambda e, c=c: e.activation(out=WO[:, c, :], in_=WOf[c % 2][:], func=AF.Identity), r=[f'WOf{c % 2}'], w=[('WO', c)])
            if debug:
                dmx = sb("dmx", [128, T], stack=m3)
                for c in range(8):
                    P.op('dve', lambda e, c=c: e.tensor_copy(out=dmx[:], in_=MIXIN[:, c, :]), w=['dmx'])
                    P.dma('sp', lambda e, c=c: e.dma_start(out=dbg["dbg_mixin"][:, c * T:(c + 1) * T], in_=dmx[:]), r=['dmx'])
            for ti in range(NT):
                P.dma('act', lambda e, ti=ti: e.dma_start(out=xts[ti][:], in_=x_d[ti * 128:(ti + 1) * 128, :]), w=[f'xtm{ti}'])
            for ti in range(NT):
                k = ti % 4
                mt = mixt[k]; xt = xts[ti]
                for half in range(2):
                    bi = (ti * 2 + half) % 4
                    bank = pbanks[bi]
                    for c in range(8):
                        P.op('pe', lambda e, c=c, ti=ti, half=half, bank=bank: e.matmul(bank[:], MIXIN[:, c, ti * 128:(ti + 1) * 128], WO[:, c, half * 512:(half + 1) * 512],
                                                                                        start=(c == 0), stop=(c == 7)),
                             r=[('WO', c)], w=[PB_[bi]], sig=(c == 7))
                    P.op('act', lambda e, mt=mt, half=half, bank=bank: e.activation(out=mt[:, half * 512:(half + 1) * 512], in_=bank[:], func=AF.Identity),
                         r=[PB_[bi]], w=[f'mixt{k}'])
                P.op('act', lambda e, mt=mt: e.activation(out=jk[:], in_=mt[:], func=AF.Square, accum_out=ssq[:]), r=[f'mixt{k}'], w=['jk', 'pn_ss'])
                P.op('act', lambda e: e.activation(out=rr[:], in_=ssq[:], func=AF.Ln, scale=1.0 / D, bias=EPS), r=['pn_ss'], w=['pn_rr'])
                P.op('act', lambda e: e.activation(out=rr[:], in_=rr[:], func=AF.Exp, scale=-0.5), r=['pn_rr'], w=['pn_rr'])
                P.op('dve', lambda e, mt=mt: e.scalar_tensor_tensor(out=mt[:], in0=mt[:], scalar=rr[:, 0:1], in1=GP1[:], op0=ALU.mult, op1=ALU.mult),
                     r=[f'mixt{k}', 'pn_rr'], w=[f'mixt{k}'])
                P.op('dve', lambda e, mt=mt, xt=xt: e.tensor_tensor(out=mt[:], in0=mt[:], in1=xt[:], op=ALU.add), r=[f'mixt{k}', f'xtm{ti}'], w=[f'mixt{k}'])
                P.dma('sp', lambda e, ti=ti, mt=mt: e.dma_start(out=y_d[ti * 128:(ti + 1) * 128, :], in_=mt[:]), r=[f'mixt{k}'], w=[('y', ti)])
                if debug:
                    P.dma('sp', lambda e, ti=ti, mt=mt: e.dma_start(out=dbg["dbg_h"][ti * 128:(ti + 1) * 128, :], in_=mt[:]), r=[f'mixt{k}'])
            P.barrier()
            P.emit(block)


_NC_CACHE = {}


def make_in_maps(inp):
    f = lambda a: np.ascontiguousarray(np.asarray(a, dtype=np.float32))
    l = 0
    shared = {
        "ada_w": f(inp["ada_w"][l]),
        "ada_b_col": f(inp["ada_b"][l].reshape(48, 128).T),
        "ada_b_row": f(inp["ada_b"][l].reshape(1, 6 * D)),
        "mix_pre_g": f(inp["mix_pre_g"][l].reshape(8, 128).T),
        "ffn_pre_g": f(inp["ffn_pre_g"][l].reshape(8, 128).T),
        "mix_post_g": f(inp["mix_post_g"][l].reshape(1, D)),
        "ffn_post_g": f(inp["ffn_post_g"][l].reshape(1, D)),
        "w_in": f(inp["w_in"][l]),
        "w_alpha": f(inp["w_alpha"][l]),
        "b_alpha": f(inp["b_alpha"][l].reshape(4, 64).T),
        "gla_norm_g": f(inp["gla_norm_g"][l].reshape(128, 1)),
        "s5_lre": f(inp["s5_lambda_re"][l].reshape(16, 128).T),
        "s5_lim": f(inp["s5_lambda_im"][l].reshape(16, 128).T),
        "s5_ldt": f(np.repeat(np.asarray(inp["s5_log_dt"][l]), 64).reshape(16, 128).T),
        "s5_bre": f(np.asarray(inp["s5_b_re"][l]).reshape(16, 128, 16).transpose(1, 0, 2)),
        "s5_bim": f(np.asarray(inp["s5_b_im"][l]).reshape(16, 128, 16).transpose(1, 0, 2)),
        "s5_cre": f(np.asarray(inp["s5_c_re"][l]).transpose(0, 2, 1).reshape(16, 128, 16).transpose(1, 0, 2)),
        "s5_cim": f(np.asarray(inp["s5_c_im"][l]).transpose(0, 2, 1).reshape(16, 128, 16).transpose(1, 0, 2)),
        "s5_d": f(np.asarray(inp["s5_d"][l]).reshape(4, 128).T),
        "s5_glu_w": f(inp["s5_glu_w"][l]),
        "s5_glu_b": f(np.asarray(inp["s5_glu_b"][l]).reshape(4, 128).T),
        "w_out": f(inp["w_out"][l]),
        "router_w": f(inp["router_w"][l]),
        "router_b": f(np.asarray(inp["router_b"][l]).reshape(1, NE)),
        "exp_w_gu": f(inp["exp_w_gu"][l]),
        "exp_b_gu": f(np.asarray(inp["exp_b_gu"][l]).reshape(NE, 16, 128).transpose(2, 0, 1)),
        "exp_w_down": f(inp["exp_w_down"][l]),
        "exp_b_down": f(inp["exp_b_down"][l]),
    }
    x = np.asarray(inp["x"], dtype=np.float32)
    c = np.asarray(inp["c"], dtype=np.float32)
    maps = []
    for b in range(x.shape[0]):
        m = dict(shared)
        m["x"] = np.ascontiguousarray(x[b])
        m["c128"] = np.ascontiguousarray(c[b].reshape(8, 128).T)
        maps.append(m)
    return maps


def kernel(**inputs):
    maps = make_in_maps(inputs)
    if "nc" not in _NC_CACHE:
        _NC_CACHE["nc"] = build()
    nc = _NC_CACHE["nc"]
    res = run_bass_kernel_spmd(nc, maps, core_ids=list(range(len(maps))))
    out = np.stack([np.asarray(r["y"], dtype=np.float32) for r in res.results], axis=0)
    return out
```

```python
import math
from contextlib import ExitStack
import numpy as np
import concourse.bass as bass
import concourse.mybir as mybir
from concourse.bass_utils import run_bass_kernel_spmd

F32 = mybir.dt.float32
BF16 = mybir.dt.bfloat16
I32 = mybir.dt.int32
AF = mybir.ActivationFunctionType
ALU = mybir.AluOpType

D = 1024
T = 2048
NT = T // 128
NE = 32
EPS = 1e-6
PHASES = ("ada", "mixer", "moe")


class Prog:
    def __init__(self, nc, same_engine_sync=True, ndma_sems=12):
        self.nc = nc
        self.names = ('pe', 'act', 'dve', 'pool', 'sp')
        self.q = {e: [] for e in self.names}
        self.cnt = {e: 0 for e in self.names}
        self.sem = {}
        self.dsem = {}
        self.dcnt = {e: 0 for e in self.names}
        self.ndma = ndma_sems
        self.last_w = {}
        self.readers = {}
        self.waited = {e: {} for e in self.names}
        self.same = same_engine_sync
        self.recent_dma = {e: [] for e in self.names}
        self.last_tok = {}
        self.gcnt = 0
        self.gtoks = []
        self.gdma = 6

    def alloc_sems(self, stack):
        for e in self.names:
            self.sem[e] = stack.enter_context(self.nc.semaphore(f"s_{e}"))
        for e in ('sp', 'pool', 'act', 'grp'):
            self.dsem[e] = [stack.enter_context(self.nc.semaphore(f"d_{e}{i}")) for i in range(self.ndma)]

    def _deps(self, r, w):
        toks = []
        for k in r:
            if k in self.last_w:
                toks.append(self.last_w[k])
        for k in w:
            if k in self.last_w:
                toks.append(self.last_w[k])
            toks.extend(self.readers.get(k, []))
        return toks

    def _waits(self, e, toks):
        need = {}
        for (skey, val, src_e, kind) in toks:
            if kind == 'nosig' and src_e == e:
                continue
            if kind != 'dma' and src_e == e and not self.same:
                continue
            if self.waited[e].get(skey, 0) >= val:
                continue
            if need.get(skey, 0) < val:
                need[skey] = val
        out = []
        for skey, val in need.items():
            self.waited[e][skey] = val
            out.append((skey, val))
        return out

    def _semobj(self, skey):
        if skey[0] == 'c':
            return self.sem[skey[1]]
        return self.dsem[skey[1]][skey[2]]

    def _record(self, tok, r, w):
        for k in r:
            self.readers.setdefault(k, []).append(tok)
        for k in w:
            self.last_w[k] = tok
            self.readers[k] = []

    def op(self, e, fn, r=(), w=(), sig=True):
        toks = self._deps(r, w)
        waits = self._waits(e, toks)
        if sig:
            self.cnt[e] += 1
            idx = self.cnt[e]
        else:
            idx = self.cnt[e] + 1
        tok = (('c', e), idx, e, 'sig' if sig else 'nosig')
        self.q[e].append((fn, waits, (('c', e), 1) if sig else None))
        self._record(tok, r, w)
        if sig:
            self.last_tok[e] = tok
        return tok

    def dma(self, e, fn, r=(), w=(), grp=None):
        toks = self._deps(r, w)
        if grp is None:
            j = self.dcnt[e]
            self.dcnt[e] += 1
            qn = e
        else:
            j = self.gcnt
            self.gcnt += 1
            qn = 'grp'
        nd = self.ndma if grp is None else self.gdma
        slot = j % nd
        val = 16 * (j // nd + 1)
        skey = ('d', qn, slot)
        if j >= nd:
            toks.append((skey, val - 16, e, 'dma'))
        waits = self._waits(e, toks)
        tok = (skey, val, e, 'dma')
        self.q[e].append((fn, waits, (skey, 16)))
        self._record(tok, r, w)
        if grp is None:
            self.recent_dma[e].append(tok)
            self.recent_dma[e] = self.recent_dma[e][-self.ndma:]
        else:
            self.gtoks.append(tok)
            self.gtoks = self.gtoks[-self.gdma:]
        return tok

    def wait_group(self, engines):
        for e in engines:
            waits = self._waits(e, list(self.gtoks))
            self.q[e].append((None, waits, None))

    def last_w_merge(self, key, other):
        pass

    def barrier(self):
        toks = [t for t in self.last_tok.values()]
        for e in self.names:
            toks.extend(self.recent_dma[e])
        for e in self.names:
            waits = self._waits(e, [t for t in toks if not (t[3] != 'dma' and t[2] == e)])
            self.q[e].append((None, waits, None))
        self.last_w = {}
        self.readers = {}

    def emit(self, block):
        prog = self

        def run(e):
            items = prog.q[e]
            prog.q[e] = []

            def body(engine):
                for fn, waits, inc in items:
                    for skey, val in waits:
                        engine.wait_ge(prog._semobj(skey), val)
                    if fn is None:
                        continue
                    ins = fn(engine)
                    if inc is not None:
                        ins.then_inc(prog._semobj(inc[0]), inc[1])
            return body
        block.tensor(run('pe'))
        block.scalar(run('act'))
        block.vector(run('dve'))
        block.gpsimd(run('pool'))
        block.sync(run('sp'))


def build(debug=False, phases=PHASES, n_exp=NE):
    nc = bass.Bass("TRN2", target_bir_lowering=False)

    def din(name, shape, dt=F32):
        return nc.dram_tensor(name, list(shape), dt, kind="ExternalInput").ap()

    x_d = din("x", [T, D])
    c_d = din("c128", [128, 8])
    adaw_d = din("ada_w", [D, 6 * D])
    adab_col_d = din("ada_b_col", [128, 48])
    adab_row_d = din("ada_b_row", [1, 6 * D])
    preg1_d = din("mix_pre_g", [128, 8])
    preg2_d = din("ffn_pre_g", [128, 8])
    postg1_d = din("mix_post_g", [1, D])
    postg2_d = din("ffn_post_g", [1, D])
    win_d = din("w_in", [D, 2064])
    walpha_d = din("w_alpha", [16, 256])
    balpha_d = din("b_alpha", [64, 4])
    glag_d = din("gla_norm_g", [128, 1])
    lre_d = din("s5_lre", [128, 16])
    lim_d = din("s5_lim", [128, 16])
    ldt_d = din("s5_ldt", [128, 16])
    bre_d = din("s5_bre", [128, 16, 16])
    bim_d = din("s5_bim", [128, 16, 16])
    cre_d = din("s5_cre", [128, 16, 16])
    cim_d = din("s5_cim", [128, 16, 16])
    s5d_d = din("s5_d", [128, 4])
    gluw_d = din("s5_glu_w", [512, 512])
    glub_d = din("s5_glu_b", [128, 4])
    wout_d = din("w_out", [D, D])
    rw_d = din("router_w", [D, NE])
    rb_d = din("router_b", [1, NE])
    wgu_d = din("exp_w_gu", [NE, D, 2 * D])
    bgu_d = din("exp_b_gu", [128, NE, 16])
    wdn_d = din("exp_w_down", [NE, D, D])
    bdn_d = din("exp_b_down", [NE, D])
    y_d = nc.dram_tensor("y", [T, D], F32, kind="ExternalOutput").ap()
    wgb_d = nc.dram_tensor("wgb_scratch", [NE * 128, 8, 2 * D], BF16).ap()
    wdb_d = nc.dram_tensor("wdb_scratch", [NE * 128, 8, D], BF16).ap()
    dbg = {}
    if debug:
        for nm, shp in (("dbg_modc", [128, 32]), ("dbg_gp", [128, 2048]), ("dbg_hn", [128, 8 * T]),
                        ("dbg_mixin", [128, 8 * T]), ("dbg_h", [T, D]), ("dbg_G", [128, NT * NE]),
                        ("dbg_acc", [128, NT * D])):
            dbg[nm] = nc.dram_tensor(nm, shp, F32, kind="ExternalOutput").ap()

    with ExitStack() as st:
        P = Prog(nc)

        _names = {}

        def sb(name, shape, dt=F32, stack=None):
            _names[name] = _names.get(name, 0) + 1
            if _names[name] > 1:
                name = f"{name}_v{_names[name]}"
            return (stack or st).enter_context(nc.sbuf_tensor(name, list(shape), dt))

        ident = sb("ident", [128, 128])
        identb = sb("identb", [128, 128], BF16)
        ones_b = sb("ones_b", [128, 128], BF16)
        S1 = sb("S1", [128, 8]); B1 = sb("B1", [128, 8]); S2 = sb("S2", [128, 8]); B2 = sb("B2", [128, 8])
        GP2 = sb("GP2", [128, D])
        gp1_stack = ExitStack()
        GP1 = sb("GP1", [128, D], stack=gp1_stack)
        pbanks = [st.enter_context(nc.psum_tensor(f"pb{i}", [128, 512], F32)) for i in range(8)]
        P.alloc_sems(st)
        block = st.enter_context(nc.Block())

        P.op('pool', lambda e: e.memset(ident[:], 0.0), w=['ident'])
        P.op('pool', lambda e: e.affine_select(out=ident[:], in_=ident[:], pattern=[[-1, 128]], compare_op=ALU.not_equal,
                                               fill=1.0, base=0, channel_multiplier=1), r=['ident'], w=['ident'])
        P.op('dve', lambda e: e.tensor_copy(out=identb[:], in_=ident[:]), r=['ident'], w=['identb'])
        P.op('dve', lambda e: e.memset(ones_b[:], 1.0), w=['ones_b'])

        xs_d = nc.dram_tensor("xs_scratch", [63 * 256, D], BF16).ap()

        def dbg_dump(name, src_ap, keys, cols=None):
            if not debug:
                return
            d = dbg[name] if cols is None else dbg[name][:, cols[0]:cols[1]]
            P.dma('sp', lambda e: e.dma_start(out=d, in_=src_ap), r=keys)

        def issue_cast_pass(e_lo=0, e_hi=NE):
            wgr = wgu_d.rearrange("e r n -> (e r) n")
            wdr = wdn_d.rearrange("e r n -> (e r) n")
            for ei in range(e_lo, e_hi):
                for kc in range(8):
                    rs = slice((ei * 8 + kc) * 128, (ei * 8 + kc + 1) * 128)
                    ds_ = slice(ei * 128, (ei + 1) * 128)
                    P.dma('pool', lambda e, rs=rs, ds_=ds_, kc=kc: e.dma_start(out=wgb_d[ds_, kc, :], in_=wgr[rs, :]), grp='cast')
                    P.dma('pool', lambda e, rs=rs, ds_=ds_, kc=kc: e.dma_start(out=wdb_d[ds_, kc, :], in_=wdr[rs, :]), grp='cast')

        with ExitStack() as pa:
            c_t = sb("c_t", [128, 8], stack=pa)
            sc_t = sb("sc_t", [128, 8], stack=pa)
            scb = sb("scb", [128, 8, 128], stack=pa)
            adab_c = sb("adab_c", [128, 48], stack=pa)
            pg1 = sb("pg1", [128, 8], stack=pa); pg2 = sb("pg2", [128, 8], stack=pa)
            abr = sb("abr", [128, 2, D], stack=pa)
            pgr = sb("pgr", [128, 2, D], stack=pa)
            modc = sb("modc", [128, 32], stack=pa)
            aw = [sb(f"aw{i}", [128, 6 * D], stack=pa) for i in range(2)]
            ZT = sb("ZT", [128, D], BF16, stack=pa)
            P.op('dve', lambda e: e.memset(ZT[:], 0.0), w=['ZT'])
            P.dma('sp', lambda e: e.dma_start(out=c_t[:], in_=c_d[:, :]), w=['c_t'])
            P.dma('sp', lambda e: e.dma_start(out=adab_c[:], in_=adab_col_d[:, :]), w=['adab_c'])
            P.dma('sp', lambda e: e.dma_start(out=pg1[:], in_=preg1_d[:, :]), w=['pg1'])
            P.dma('sp', lambda e: e.dma_start(out=pg2[:], in_=preg2_d[:, :]), w=['pg2'])
            P.dma('pool', lambda e: e.dma_start(out=abr[:, 0, :], in_=adab_row_d[0:1, 2 * D:3 * D].partition_broadcast(128)), w=['abr0'])
            P.dma('pool', lambda e: e.dma_start(out=abr[:, 1, :], in_=adab_row_d[0:1, 5 * D:6 * D].partition_broadcast(128)), w=['abr1'])
            P.dma('pool', lambda e: e.dma_start(out=pgr[:, 0, :], in_=postg1_d[0:1, :].partition_broadcast(128)), w=['pgr0'])
            P.dma('pool', lambda e: e.dma_start(out=pgr[:, 1, :], in_=postg2_d[0:1, :].partition_broadcast(128)), w=['pgr1'])
            P.op('act', lambda e: e.activation(out=sc_t[:], in_=c_t[:], func=AF.Silu), r=['c_t'], w=['sc_t'])
            xs_v0 = xs_d.rearrange("(a p) d -> p a d", p=128)
            for a9 in range(14):
                P.dma('act', lambda e, a9=a9: e.dma_start(out=xs_v0[:, a9 * 9:(a9 + 1) * 9, :], in_=ZT[:].unsqueeze(1).to_broadcast([128, 9, D])),
                      r=['ZT'], w=[('xsz', a9)])
            P.op('dve', lambda e: e.tensor_copy(out=scb[:], in_=sc_t[:].unsqueeze(2).to_broadcast([128, 8, 128])), r=['sc_t'], w=['scb'])
            colchunks = list(range(0, 16)) + list(range(24, 40))
            pcol = pbanks[0]
            prow = pbanks[1:5]
            first = True
            for kc in range(8):
                b = kc % 2
                P.dma('sp', lambda e, kc=kc, b=b: e.dma_start(out=aw[b][:], in_=adaw_d[kc * 128:(kc + 1) * 128, :]), w=[f'aw{b}'])
                for i, m in enumerate(colchunks):
                    last = (kc == 7 and i == len(colchunks) - 1)
                    P.op('pe', lambda e, b=b, m=m, i=i, kc=kc, first=first, last=last: e.matmul(
                        pcol[:, i:i + 1], aw[b][:, m * 128:(m + 1) * 128], sc_t[:, kc:kc + 1], start=first, stop=last),
                        r=[f'aw{b}', 'sc_t'], w=['pcol'], sig=(i == len(colchunks) - 1))
                    first = False
                for q in range(4):
                    col0 = (2 * D if q < 2 else 5 * D) + (q % 2) * 512
                    P.op('pe', lambda e, b=b, q=q, kc=kc, col0=col0: e.matmul(
                        prow[q][:], scb[:, kc, :], aw[b][:, col0:col0 + 512], start=(kc == 0), stop=(kc == 7)),
                        r=[f'aw{b}', 'scb'], w=[f'prow{q}'])
            P.op('dve', lambda e: e.tensor_tensor(out=modc[:, 0:16], in0=pcol[:, 0:16], in1=adab_c[:, 0:16], op=ALU.add),
                 r=['pcol', 'adab_c'], w=['modc'])
            P.op('dve', lambda e: e.tensor_tensor(out=modc[:, 16:32], in0=pcol[:, 16:32], in1=adab_c[:, 24:40], op=ALU.add),
                 r=['pcol', 'adab_c'], w=['modc'])
            P.op('dve', lambda e: e.scalar_tensor_tensor(out=S1[:], in0=modc[:, 8:16], scalar=1.0, in1=pg1[:], op0=ALU.add, op1=ALU.mult),
                 r=['modc', 'pg1'], w=['S1'])
            P.op('dve', lambda e: e.tensor_copy(out=B1[:], in_=modc[:, 0:8]), r=['modc'], w=['B1'])
            P.op('dve', lambda e: e.scalar_tensor_tensor(out=S2[:], in0=modc[:, 24:32], scalar=1.0, in1=pg2[:], op0=ALU.add, op1=ALU.mult),
                 r=['modc', 'pg2'], w=['S2'])
            P.op('dve', lambda e: e.tensor_copy(out=B2[:], in_=modc[:, 16:24]), r=['modc'], w=['B2'])
            for q in range(4):
                gp = GP1 if q < 2 else GP2
                j = q // 2
                sl = slice((q % 2) * 512, (q % 2) * 512 + 512)
                P.op('dve', lambda e, gp=gp, j=j, sl=sl, q=q: e.tensor_tensor(out=gp[:, sl], in0=prow[q][:], in1=abr[:, j, sl], op=ALU.add),
                     r=[f'prow{q}', f'abr{j}'], w=[f'gp{q}'])
                P.op('dve', lambda e, gp=gp, j=j, sl=sl, q=q: e.tensor_tensor(out=gp[:, sl], in0=gp[:, sl], in1=pgr[:, j, sl], op=ALU.mult),
                     r=[f'gp{q}', f'pgr{j}'], w=[f'gp{q}'])
            if debug:
                dbg_dump("dbg_modc", modc[:], ['modc'])
                dbg_dump("dbg_gp", GP1[:], ['gp0', 'gp1'], cols=(0, 1024))
                dbg_dump("dbg_gp", GP2[:], ['gp2', 'gp3'], cols=(1024, 2048))
            P.barrier()
            P.emit(block)

        def prenorm_tile(src, src_keys, ti, HN, hn_key, Sc, Bc, tmp, pst, f32copy=None, sx=''):
            ssq, rr, xs = tmp
            P.op('act', lambda e: e.activation(out=xs[:], in_=src, func=AF.Square, accum_out=ssq[:]), r=src_keys, w=['pn_xs' + sx, 'pn_ss' + sx])
            P.op('act', lambda e: e.activation(out=rr[:], in_=ssq[:], func=AF.Ln, scale=1.0 / D, bias=EPS), r=['pn_ss' + sx], w=['pn_rr' + sx])
            P.op('act', lambda e: e.activation(out=rr[:], in_=rr[:], func=AF.Exp, scale=-0.5), r=['pn_rr' + sx], w=['pn_rr' + sx])
            P.op('dve', lambda e: e.tensor_scalar(out=xs[:], in0=src, scalar1=rr[:, 0:1], scalar2=None, op0=ALU.mult), r=src_keys + ['pn_rr' + sx], w=['pn_xs' + sx])
            for half in range(2):
                pt = pst[half]
                for q in range(4):
                    kc = half * 4 + q
                    P.op('pe', lambda e, pt=pt, q=q, kc=kc: e.transpose(out=pt[:, q * 128:(q + 1) * 128], in_=xs[:, kc * 128:(kc + 1) * 128], identity=ident[:]),
                         r=['pn_xs' + sx, 'ident'], w=[f'pb{half}'], sig=(q == 3))
                for q in range(4):
                    kc = half * 4 + q
                    if HN is not None:
                        P.op('act', lambda e, pt=pt, q=q, kc=kc: e.activation(out=HN[:, kc, ti * 128:(ti + 1) * 128], in_=pt[:, q * 128:(q + 1) * 128],
                                                                              func=AF.Identity, scale=Sc[:, kc:kc + 1], bias=Bc[:, kc:kc + 1]),
                             r=[f'pb{half}'], w=[hn_key(kc, ti)])
                    if f32copy is not None:
                        P.op('act', lambda e, pt=pt, q=q, kc=kc: e.activation(out=f32copy[:, kc, :], in_=pt[:, q * 128:(q + 1) * 128],
                                                                              func=AF.Identity, scale=Sc[:, kc:kc + 1], bias=Bc[:, kc:kc + 1]),
                             r=[f'pb{half}'], w=['pn_f32' + sx])

        build_rest(nc, P, st, block, locals(), debug, phases, n_exp)
    return nc


def build_rest(nc, P, st, block, L, debug, phases, n_exp):
    sb = L['sb']; pbanks = L['pbanks']; ident = L['ident']; identb = L['identb']; ones_b = L['ones_b']
    S1, B1, S2, B2, GP1, GP2 = L['S1'], L['B1'], L['S2'], L['B2'], L['GP1'], L['GP2']
    x_d, y_d = L['x_d'], L['y_d']
    dbg = L['dbg']; dbg_dump = L['dbg_dump']; prenorm_tile = L['prenorm_tile']
    rw_d, rb_d, wgu_d, bgu_d, wdn_d, bdn_d = L['rw_d'], L['rb_d'], L['wgu_d'], L['bgu_d'], L['wdn_d'], L['bdn_d']

    if "mixer" in phases:
        import_mixer = build_mixer
        import_mixer(nc, P, st, block, L, debug)
    else:
        L['issue_cast_pass'](0, NE)
        with ExitStack() as pb:
            xt = sb("xt_cp", [128, D], stack=pb)
            for ti in range(NT):
                P.dma('sp', lambda e, ti=ti: e.dma_start(out=xt[:], in_=x_d[ti * 128:(ti + 1) * 128, :]), w=['xt_cp'])
                P.dma('sp', lambda e, ti=ti: e.dma_start(out=y_d[ti * 128:(ti + 1) * 128, :], in_=xt[:]), r=['xt_cp'], w=[('y', ti)])
            P.barrier()
            P.emit(block)

    L['gp1_stack'].close()
    if 'moe' not in phases:
        return
    NB = 63
    BS = 256
    NSLOT = NB * BS
    X = mybir.AxisListType.X
    xs_d = L['xs_d']
    assert NSLOT == 63 * 256
    ys_d = nc.dram_tensor("ys_scratch", [NSLOT, D], F32).ap()
    wgu_rows = L['wgb_d']
    wdn_rows = L['wdb_d']
    bgu_rows = bgu_d.rearrange("p e c -> (p e) c")
    IOA = bass.IndirectOffsetOnAxis
    with ExitStack() as pc:
        G = sb("G", [128, NT, NE], stack=pc)
        SLI = sb("SLI", [128, NT, 4], I32, stack=pc)
        GK = sb("GK", [128, NT, 4], stack=pc)
        OFFW = sb("OFFW", [128, NB], I32, stack=pc)
        OFFB = sb("OFFB", [128, NB], I32, stack=pc)
        BD = sb("BD", [NE, D], stack=pc)
        P.dma('sp', lambda e: e.dma_start(out=BD[:], in_=bdn_d[:, :]), w=['BD'])

        with ExitStack() as c1:
            HR = sb("HR", [128, NT, D], BF16, stack=c1)
            RW = sb("RW", [128, 8, NE], stack=c1)
            RB = sb("RB", [128, NE], stack=c1)
            S2R = sb("S2R", [128, D], stack=c1)
            B2R = sb("B2R", [128, D], stack=c1)
            bct = sb("bct", [128, 128], stack=c1)
            MK = sb("MK", [128, NT, NE], BF16, stack=c1)
            LT = sb("LT", [128, 128], stack=c1)
            LTb = sb("LTb", [128, 128], BF16, stack=c1)
            POS = sb("POS", [128, NT, NE], stack=c1)
            SM = sb("SM", [128, NT, NE], stack=c1)
            CNT = sb("CNT", [128, NE], stack=c1)
            NBK = sb("NBK", [128, NE], stack=c1)
            NBI = sb("NBI", [128, NE], I32, stack=c1)
            tq = sb("tq", [128, NE], stack=c1)
            CS = sb("CS", [128, NE], stack=c1)
            PST = sb("PST", [128, NE], stack=c1)
            PEND = sb("PEND", [128, NE], stack=c1)
            one32 = sb("one32", [128, NE], stack=c1)
            SL4 = sb("SL4", [128, NT, 4], stack=c1)
            eq4 = sb("eq4", [128, 4, NE], stack=c1)
            BTHI = sb("BTHI", [128, NB, NE], I32, stack=c1)
            BTH = sb("BTH", [128, NB, NE], stack=c1)
            EB = sb("EB", [128, NB], stack=c1)
            SK = sb("SK", [128, NB], stack=c1)
            KCI = sb("KCI", [128, NB], I32, stack=c1)
            KCF = sb("KCF", [128, NB], stack=c1)
            PI = sb("PI", [128, NB], I32, stack=c1)
            PF = sb("PF", [128, NB], stack=c1)
            P.dma('sp', lambda e: e.dma_start(out=RW[:], in_=rw_d.rearrange("(kc p) n -> p kc n", p=128)), w=['RW'])
            P.dma('pool', lambda e: e.dma_start(out=RB[:], in_=rb_d[0:1, :].partition_broadcast(128)), w=['RB'])
            pr = [pbanks[6], pbanks[7]]
            for vi, (col, rowt, nm) in enumerate(((S2, S2R, 'S2R'), (B2, B2R, 'B2R'))):
                for kc in range(8):
                    P.op('dve', lambda e, col=col, kc=kc: e.tensor_copy(out=bct[:], in_=col[:, kc:kc + 1].to_broadcast([128, 128])), w=['bct'])
                    bank = pr[kc // 4]
                    P.op('pe', lambda e, bank=bank, kc=kc: e.matmul(bank[:, (kc % 4) * 128:(kc % 4 + 1) * 128], bct[:], ident[:], start=True, stop=True),
                         r=['bct', 'ident'], w=[f'pb{6 + kc // 4}'])
                    if kc % 4 == 3:
                        h_ = kc // 4
                        P.op('act', lambda e, bank=bank, rowt=rowt, h_=h_: e.activation(out=rowt[:, h_ * 512:(h_ + 1) * 512], in_=bank[:], func=AF.Identity),
                             r=[f'pb{6 + h_}'], w=[nm])
            pst = [pbanks[0], pbanks[1]]
            plg = pbanks[2]
            dbl = {}
            for nm_, shp_ in (("ht", [128, D]), ("pn_ss", [128, 1]), ("pn_rr", [128, 1]), ("pn_xs", [128, D]), ("hn32", [128, 8, 128]), ("lg", [128, NE]),
                              ("top8", [128, 8]), ("nm1", [128, 1]), ("msk", [128, NE]), ("ex", [128, NE]), ("ssum", [128, 1])):
                dbl[nm_] = [sb(nm_ + "_d%d" % i, shp_, stack=c1) for i in range(4 if nm_ == "ht" else 2)]
            for ti in range(NT):
                k2 = ti % 2
                sx = str(k2)
                ht_, ssq_, rr_, xs_, hn32_, lg_, top8_, nm1_, msk_, ex_, ssum_ = [dbl[n][k2] for n in ("ht", "pn_ss", "pn_rr", "pn_xs", "hn32", "lg", "top8", "nm1", "msk", "ex", "ssum")]
                ht_ = dbl["ht"][ti % 4]
                hx = 'h' + str(ti % 4)
                P.dma('sp', lambda e, ti=ti, ht_=ht_: e.dma_start(out=ht_[:], in_=y_d[ti * 128:(ti + 1) * 128, :]), r=[('y', ti)], w=['ht' + hx])
                prenorm_tile(ht_[:], ['ht' + hx], ti, None, None, S2, B2, (ssq_, rr_, xs_), pst, f32copy=hn32_, sx=sx)
                P.op('dve', lambda e, ht_=ht_, xs_=xs_: e.tensor_tensor(out=ht_[:], in0=xs_[:], in1=S2R[:], op=ALU.mult), r=['pn_xs' + sx, 'S2R'], w=['ht' + hx])
                P.op('dve', lambda e, ti=ti, ht_=ht_: e.tensor_tensor(out=HR[:, ti, :], in0=ht_[:], in1=B2R[:], op=ALU.add), r=['ht' + hx, 'B2R'], w=[('HR', ti)])
                for kc in range(8):
                    P.op('pe', lambda e, kc=kc, hn32_=hn32_: e.matmul(plg[:, 0:NE], hn32_[:, kc, :], RW[:, kc, :], start=(kc == 0), stop=(kc == 7)),
                         r=['pn_f32' + sx, 'RW'], w=['plg'], sig=(kc == 7))
                P.op('dve', lambda e, lg_=lg_: e.tensor_tensor(out=lg_[:], in0=plg[:, 0:NE], in1=RB[:], op=ALU.add), r=['plg', 'RB'], w=['lg' + sx])
                P.op('dve', lambda e, lg_=lg_, top8_=top8_: e.max(out=top8_[:], in_=lg_[:]), r=['lg' + sx], w=['top8' + sx])
                P.op('dve', lambda e, top8_=top8_, nm1_=nm1_: e.tensor_scalar(out=nm1_[:], in0=top8_[:, 0:1], scalar1=-1.0, scalar2=None, op0=ALU.mult), r=['top8' + sx], w=['nm1' + sx])
                P.op('dve', lambda e, lg_=lg_, top8_=top8_, msk_=msk_: e.tensor_scalar(out=msk_[:], in0=lg_[:], scalar1=top8_[:, 3:4], scalar2=None, op0=ALU.is_ge),
                     r=['lg' + sx, 'top8' + sx], w=['msk' + sx])
                P.op('dve', lambda e, ti=ti, msk_=msk_: e.tensor_copy(out=MK[:, ti, :], in_=msk_[:]), r=['msk' + sx], w=[('MK', ti)])
                P.op('act', lambda e, lg_=lg_, nm1_=nm1_, ex_=ex_: e.activation(out=ex_[:], in_=lg_[:], func=AF.Exp, bias=nm1_[:, 0:1], scale=1.0), r=['lg' + sx, 'nm1' + sx], w=['ex' + sx])
                P.op('dve', lambda e, ex_=ex_, msk_=msk_: e.tensor_tensor(out=ex_[:], in0=ex_[:], in1=msk_[:], op=ALU.mult), r=['ex' + sx, 'msk' + sx], w=['ex' + sx])
                P.op('dve', lambda e, ex_=ex_, ssum_=ssum_: e.reduce_sum(out=ssum_[:], in_=ex_[:], axis=X), r=['ex' + sx], w=['ssum' + sx])
                P.op('dve', lambda e, ssum_=ssum_: e.reciprocal(out=ssum_[:], in_=ssum_[:]), r=['ssum' + sx], w=['ssum' + sx])
                P.op('dve', lambda e, ti=ti, ex_=ex_, ssum_=ssum_: e.tensor_scalar(out=G[:, ti, :], in0=ex_[:], scalar1=ssum_[:, 0:1], scalar2=None, op0=ALU.mult),
                     r=['ex' + sx, 'ssum' + sx], w=[('G', ti)])
            if debug:
                dbg_dump("dbg_G", G[:].rearrange("p a b -> p (a b)"), [('G', ti) for ti in range(NT)])
            top8 = dbl['top8'][0]
            GKEYS = [('G', ti) for ti in range(NT)]
            MKEYS = [('MK', ti) for ti in range(NT)]
            P.op('pool', lambda e: e.memset(LT[:], 1.0), w=['LT'])
            P.op('pool', lambda e: e.affine_select(out=LT[:], in_=LT[:], pattern=[[1, 128]], compare_op=ALU.is_ge, fill=0.0, base=-1, channel_multiplier=-1),
                 r=['LT'], w=['LT'])
            P.op('dve', lambda e: e.tensor_copy(out=LTb[:], in_=LT[:]), r=['LT'], w=['LTb'])
            ppos = pbanks[3]
            pcnt = pbanks[4]
            for ti in range(NT):
                for tj in range(ti + 1):
                    lt = ones_b if tj < ti else LTb
                    P.op('pe', lambda e, ti=ti, tj=tj, lt=lt: e.matmul(ppos[:, ti * NE:(ti + 1) * NE], lt[:], MK[:, tj, :], start=(tj == 0), stop=(tj == ti)),
                         r=[('MK', tj), 'LTb', 'ones_b'], w=['pb3'], sig=(tj == ti))
            for tj in range(NT):
                P.op('pe', lambda e, tj=tj: e.matmul(pcnt[:, 0:NE], ones_b[:], MK[:, tj, :], start=(tj == 0), stop=(tj == NT - 1)),
                     r=[('MK', tj), 'ones_b'], w=['pb4'], sig=(tj == NT - 1))
            dv = lambda fn, r, w: P.op('dve', fn, r=r, w=w)
            dv(lambda e: e.tensor_copy(out=POS[:].rearrange("p a b -> p (a b)"), in_=ppos[:]), ['pb3'], ['POS'])
            dv(lambda e: e.tensor_copy(out=CNT[:], in_=pcnt[:, 0:NE]), ['pb4'], ['CNT'])
            dv(lambda e: e.tensor_scalar(out=tq[:], in0=CNT[:], scalar1=float(BS - 1), scalar2=1.0 / BS, op0=ALU.add, op1=ALU.mult), ['CNT'], ['tq'])
            dv(lambda e: e.tensor_copy(out=NBI[:], in_=tq[:]), ['tq'], ['NBI'])
            dv(lambda e: e.tensor_copy(out=NBK[:], in_=NBI[:]), ['NBI'], ['NBK'])
            dv(lambda e: e.tensor_tensor(out=CS[:], in0=NBK[:], in1=tq[:], op=ALU.is_gt), ['NBK', 'tq'], ['CS'])
            dv(lambda e: e.tensor_tensor(out=NBK[:], in0=NBK[:], in1=CS[:], op=ALU.subtract), ['NBK', 'CS'], ['NBK'])
            dv(lambda e: e.memset(one32[:], 1.0), [], ['one32'])
            dv(lambda e: e.tensor_tensor_scan(out=CS[:], data0=one32[:], data1=NBK[:], initial=0.0, op0=ALU.mult, op1=ALU.add), ['one32', 'NBK'], ['CS'])
            dv(lambda e: e.tensor_scalar(out=PEND[:], in0=CS[:], scalar1=float(BS), scalar2=None, op0=ALU.mult), ['CS'], ['PEND'])
            dv(lambda e: e.tensor_tensor(out=PST[:], in0=CS[:], in1=NBK[:], op=ALU.subtract), ['CS', 'NBK'], ['PST'])
            dv(lambda e: e.tensor_scalar(out=PST[:], in0=PST[:], scalar1=float(BS), scalar2=None, op0=ALU.mult), ['PST'], ['PST'])
            dv(lambda e: e.tensor_tensor(out=POS[:], in0=POS[:], in1=PST[:].unsqueeze(1).to_broadcast([128, NT, NE]), op=ALU.add), ['POS', 'PST'], ['POS'])
            dv(lambda e: e.scalar_tensor_tensor(out=SM[:], in0=POS[:], scalar=1.0, in1=MK[:], op0=ALU.add, op1=ALU.mult), ['POS'] + MKEYS, ['SM'])
            dv(lambda e: e.tensor_scalar(out=SM[:], in0=SM[:], scalar1=-1.0, scalar2=None, op0=ALU.add), ['SM'], ['SM'])
            for ti in range(NT):
                dv(lambda e, ti=ti: e.max(out=top8[:], in_=SM[:, ti, :]), ['SM'], ['top8'])
                dv(lambda e, ti=ti: e.tensor_copy(out=SL4[:, ti, :], in_=top8[:, 0:4]), ['top8'], ['SL4'])
                dv(lambda e, ti=ti: e.tensor_tensor(out=eq4[:], in0=SM[:, ti, :].unsqueeze(1).to_broadcast([128, 4, NE]),
                                                    in1=top8[:, 0:4].unsqueeze(2).to_broadcast([128, 4, NE]), op=ALU.is_equal), ['SM', 'top8'], ['eq4'])
                dv(lambda e, ti=ti: e.tensor_tensor(out=eq4[:], in0=eq4[:], in1=G[:, ti, :].unsqueeze(1).to_broadcast([128, 4, NE]), op=ALU.mult),
                   ['eq4', ('G', ti)], ['eq4'])
                dv(lambda e, ti=ti: e.tensor_reduce(out=GK[:, ti, :], in_=eq4[:], op=ALU.add, axis=X), ['eq4'], ['GK'])
            dv(lambda e: e.tensor_copy(out=SLI[:], in_=SL4[:]), ['SL4'], ['SLI'])
            P.op('pool', lambda e: e.iota(BTHI[:], pattern=[[BS, NB], [0, NE]], base=0, channel_multiplier=0), w=['BTHI'])
            P.op('pool', lambda e: e.iota(KCI[:], pattern=[[0, NB]], base=0, channel_multiplier=1), w=['KCI'])
            P.op('pool', lambda e: e.iota(PI[:], pattern=[[0, NB]], base=0, channel_multiplier=NE), w=['PI'])
            dv(lambda e: e.tensor_copy(out=BTH[:], in_=BTHI[:]), ['BTHI'], ['BTH'])
            dv(lambda e: e.tensor_copy(out=KCF[:], in_=KCI[:]), ['KCI'], ['KCF'])
            dv(lambda e: e.tensor_copy(out=PF[:], in_=PI[:]), ['PI'], ['PF'])
            dv(lambda e: e.tensor_tensor(out=BTH[:], in0=PEND[:].unsqueeze(1).to_broadcast([128, NB, NE]), in1=BTH[:], op=ALU.is_le), ['PEND', 'BTH'], ['BTH'])
            dv(lambda e: e.tensor_reduce(out=EB[:], in_=BTH[:], op=ALU.add, axis=X), ['BTH'], ['EB'])
            dv(lambda e: e.tensor_scalar(out=EB[:], in0=EB[:], scalar1=float(NE - 1), scalar2=None, op0=ALU.min), ['EB'], ['EB'])
            dv(lambda e: e.tensor_tensor(out=PF[:], in0=PF[:], in1=EB[:], op=ALU.add), ['PF', 'EB'], ['PF'])
            dv(lambda e: e.tensor_copy(out=OFFB[:], in_=PF[:]), ['PF'], ['OFFB'])
            dv(lambda e: e.memset(SK[:], 0.0), [], ['SK'])
            dv(lambda e: e.tensor_tensor(out=SK[:, 2:NB], in0=EB[:, 2:NB], in1=EB[:, 0:NB - 2], op=ALU.is_equal), ['EB', 'SK'], ['SK'])
            dv(lambda e: e.tensor_scalar(out=SK[:], in0=SK[:], scalar1=float(1 << 22), scalar2=None, op0=ALU.mult), ['SK'], ['SK'])
            dv(lambda e: e.tensor_scalar(out=EB[:], in0=EB[:], scalar1=128.0, scalar2=None, op0=ALU.mult), ['EB'], ['EB'])
            dv(lambda e: e.tensor_tensor(out=EB[:], in0=EB[:], in1=SK[:], op=ALU.add), ['EB', 'SK'], ['EB'])
            dv(lambda e: e.tensor_tensor(out=KCF[:], in0=KCF[:], in1=EB[:], op=ALU.add), ['KCF', 'EB'], ['KCF'])
            dv(lambda e: e.tensor_copy(out=OFFW[:], in_=KCF[:]), ['KCF'], ['OFFW'])
            XSZ = []
            for ti in range(NT):
                for k in range(4):
                    P.dma('pool', lambda e, ti=ti, k=k: e.indirect_dma_start(out=xs_d[:, :], out_offset=IOA(ap=SLI[:, ti, k:k + 1], axis=0),
                                                                             in_=HR[:, ti, :], in_offset=None),
                          r=['SLI', ('HR', ti)] + (XSZ if (ti == 0 and k == 0) else []), w=[('xsw', ti, k)])
            P.barrier()
            P.emit(block)

        with ExitStack() as c2:
            WG = [sb(f"WG{i}", [128, 8, 2 * D], BF16, stack=c2) for i in range(2)]
            WD = [sb(f"WD{i}", [128, 8, D], BF16, stack=c2) for i in range(2)]
            BGB = [sb(f"BGB{i}", [128, 16], stack=c2) for i in range(2)]
            XR = [sb(f"XR{i}", [128, 2, D], BF16, stack=c2) for i in range(2)]
            XT = [sb(f"XT{i}", [128, 8, BS], BF16, stack=c2) for i in range(2)]
            ACTT = [sb(f"ACTT{i}", [128, 8, BS], BF16, stack=c2) for i in range(2)]
            YR = [sb(f"YR{i}", [128, D], stack=c2) for i in range(2)]
            gcs = [sb(f"gc{i}", [128, BS], stack=c2) for i in range(2)]
            sgs = [sb(f"sg{i}", [128, BS], stack=c2) for i in range(2)]
            u1s = [sb(f"u1{i}", [128, BS], stack=c2) for i in range(2)]
            ptbs = [pbanks[0][:].bitcast(BF16), pbanks[1][:].bitcast(BF16)]
            pgu = [pbanks[2], pbanks[3]]
            py_ = [pbanks[4], pbanks[5], pbanks[6], pbanks[7]]
            xs_b = xs_d.rearrange("(b s p) d -> b p s d", s=2, p=128)

            BREG = {}

            def _setreg(e):
                BREG['r'] = e.alloc_register('bnd_reg')
                return e.reg_mov(BREG['r'], NE * 128 - 1)
            P.q['pool'].append((_setreg, [], None))
            P.wait_group(['pool'])

            def load_gu(b):
                wb = b % 2
                P.dma('pool', lambda e, wb=wb, b=b: e.indirect_dma_start(out=WG[wb][:].rearrange("p k n -> p (k n)"), out_offset=None,
                                                                         in_=wgu_rows.rearrange("r k n -> r (k n)"),
                                                                         in_offset=IOA(ap=OFFW[:, b:b + 1], axis=0),
                                                                         bounds_check=BREG['r'], oob_is_err=False),
                      w=[('WG', wb, kc) for kc in range(8)])
                P.dma('pool', lambda e, wb=wb, b=b: e.indirect_dma_start(out=BGB[wb][:, :], out_offset=None, in_=bgu_rows[:, :],
                                                                         in_offset=IOA(ap=OFFB[:, b:b + 1], axis=0)), w=[('BGB', wb)])
                P.dma('sp', lambda e, wb=wb, b=b: e.dma_start(out=XR[wb][:], in_=xs_b[b]), w=[('XR', wb)])

            def load_down(b):
                wb = b % 2
                P.dma('pool', lambda e, wb=wb, b=b: e.indirect_dma_start(out=WD[wb][:].rearrange("p k n -> p (k n)"), out_offset=None,
                                                                         in_=wdn_rows.rearrange("r k n -> r (k n)"),
                                                                         in_offset=IOA(ap=OFFW[:, b:b + 1], axis=0),
                                                                         bounds_check=BREG['r'], oob_is_err=False),
                      w=[('WD', wb, fc) for fc in range(8)])

            n_blk = NB if n_exp >= NE else max(2, 2 * n_exp)
            cnts = {'jt': 0, 'yc': 0}

            def do_transposes(b):
                wb = b % 2
                for s_ in range(2):
                    ptb = ptbs[s_]
                    for kc in range(8):
                        P.op('pe', lambda e, s_=s_, kc=kc, wb=wb, ptb=ptb: e.transpose(out=ptb[:, kc * 128:(kc + 1) * 128], in_=XR[wb][:, s_, kc * 128:(kc + 1) * 128], identity=identb[:]),
                             r=[('XR', wb), 'identb'], w=[f'pb{s_}'], sig=(kc == 7))
                    P.op('act', lambda e, s_=s_, wb=wb, ptb=ptb: e.activation(out=XT[wb][:, :, s_ * 128:(s_ + 1) * 128], in_=ptb[:, :].rearrange("p (a b) -> p a b", b=128), func=AF.Identity),
                         r=[f'pb{s_}'], w=[('XT', wb, s_)])

            def do_gu(b):
                wb = b % 2
                for j in range(8):
                    pi = cnts['jt'] % 2
                    cnts['jt'] += 1
                    bank = pgu[pi]
                    for kc in range(8):
                        P.op('pe', lambda e, kc=kc, j=j, wb=wb, bank=bank: e.matmul(bank[:, 0:BS], WG[wb][:, kc, j * 128:(j + 1) * 128], XT[wb][:, kc, :], start=(kc == 0), stop=(kc == 7)),
                             r=[('WG', wb, kc), ('XT', wb, 0), ('XT', wb, 1)], w=[f'pb{2 + pi}'], sig=False)
                    for kc in range(8):
                        P.op('pe', lambda e, kc=kc, j=j, wb=wb, bank=bank: e.matmul(bank[:, BS:2 * BS], WG[wb][:, kc, D + j * 128:D + (j + 1) * 128], XT[wb][:, kc, :], start=(kc == 0), stop=(kc == 7)),
                             r=[('WG', wb, kc), ('XT', wb, 0), ('XT', wb, 1)], w=[f'pb{2 + pi}'], sig=(kc == 7))
                    gc, sg, u1 = gcs[pi], sgs[pi], u1s[pi]
                    P.op('dve', lambda e, gc=gc, bank=bank, j=j, wb=wb: e.tensor_scalar(out=gc[:], in0=bank[:, 0:BS], scalar1=BGB[wb][:, j:j + 1], scalar2=7.0, op0=ALU.add, op1=ALU.min),
                         r=[f'pb{2 + pi}', ('BGB', wb)], w=[f'gc{pi}'])
                    P.op('act', lambda e, gc=gc, sg=sg: e.activation(out=sg[:], in_=gc[:], func=AF.Sigmoid, scale=1.702), r=[f'gc{pi}'], w=[f'sg{pi}'])
                    P.op('dve', lambda e, u1=u1, bank=bank, j=j, wb=wb: e.tensor_scalar(out=u1[:], in0=bank[:, BS:2 * BS], scalar1=BGB[wb][:, 8 + j:9 + j], scalar2=7.0, op0=ALU.add, op1=ALU.min),
                         r=[f'pb{2 + pi}', ('BGB', wb)], w=[f'u1{pi}'])
                    P.op('dve', lambda e, u1=u1: e.tensor_scalar(out=u1[:], in0=u1[:], scalar1=-7.0, scalar2=1.0, op0=ALU.max, op1=ALU.add), r=[f'u1{pi}'], w=[f'u1{pi}'])
                    P.op('pool', lambda e, gc=gc, sg=sg: e.tensor_tensor(out=sg[:], in0=gc[:], in1=sg[:], op=ALU.mult), r=[f'gc{pi}', f'sg{pi}'], w=[f'sg{pi}'])
                    P.op('pool', lambda e, u1=u1, sg=sg, j=j, wb=wb: e.tensor_tensor(out=ACTT[wb][:, j, :], in0=u1[:], in1=sg[:], op=ALU.mult), r=[f'u1{pi}', f'sg{pi}'], w=[('ACTT', wb, j)])

            def do_down(b):
                wb = b % 2
                for s_ in range(2):
                    yr = YR[s_]
                    for half in range(2):
                        yi = cnts['yc'] % 4
                        cnts['yc'] += 1
                        for fc in range(8):
                            P.op('pe', lambda e, fc=fc, s_=s_, half=half, yi=yi, wb=wb: e.matmul(py_[yi][:], ACTT[wb][:, fc, s_ * 128:(s_ + 1) * 128], WD[wb][:, fc, half * 512:(half + 1) * 512],
                                                                                                 start=(fc == 0), stop=(fc == 7)),
                                 r=[('ACTT', wb, fc), ('WD', wb, fc)], w=[f'pb{4 + yi}'], sig=(fc == 7))
                        P.op('act', lambda e, yr=yr, half=half, yi=yi: e.activation(out=yr[:, half * 512:(half + 1) * 512], in_=py_[yi][:], func=AF.Identity),
                             r=[f'pb{4 + yi}'], w=[f'YR{s_}'])
                    r0 = b * BS + s_ * 128
                    P.dma('sp', lambda e, yr=yr, r0=r0: e.dma_start(out=ys_d[r0:r0 + 128, :], in_=yr[:]), r=[f'YR{s_}'], w=[('ys', b, s_)])

            load_gu(0)
            load_down(0)
            if n_blk > 1:
                load_gu(1)
            do_transposes(0)
            for b in range(n_blk):
                do_gu(b)
                if b + 1 < n_blk:
                    do_transposes(b + 1)
                if b >= 1:
                    do_down(b - 1)
                if b + 1 < n_blk:
                    load_down(b + 1)
                if b + 2 < n_blk:
                    load_gu(b + 2)
            do_down(n_blk - 1)
            P.barrier()
            P.emit(block)

        with ExitStack() as c3:
            hts = [sb(f"ht{i}", [128, D], stack=c3) for i in range(2)]
            Y4 = [[sb(f"Y4_{i}_{k}", [128, D], stack=c3) for k in range(4)] for i in range(2)]
            accs = [sb(f"accf{i}", [128, D], stack=c3) for i in range(2)]
            sqj = sb("sqj", [128, D], stack=c3)
            GTt = sb("GTt", [NE, 128], stack=c3)
            ssq = sb("pn_ss", [128, 1], stack=c3)
            rr = sb("pn_rr", [128, 1], stack=c3)
            pgt = pbanks[0]
            pinit = [pbanks[1], pbanks[2]]
            for ti in range(NT):
                k2 = ti % 2
                ht = hts[k2]; acc = accs[k2]
                P.dma('sp', lambda e, ti=ti, ht=ht: e.dma_start(out=ht[:], in_=y_d[ti * 128:(ti + 1) * 128, :]), w=[f'ht{k2}'])
                for k in range(4):
                    P.dma('pool', lambda e, ti=ti, k=k, k2=k2: e.indirect_dma_start(out=Y4[k2][k][:, :], out_offset=None, in_=ys_d[:, :],
                                                                                     in_offset=IOA(ap=SLI[:, ti, k:k + 1], axis=0)), w=[('Y4', k2, k)])
                P.op('pe', lambda e, ti=ti: e.transpose(out=pgt[0:NE, 0:128], in_=G[:, ti, :], identity=ident[:]), w=['pb0'])
                P.op('act', lambda e: e.activation(out=GTt[:], in_=pgt[0:NE, 0:128], func=AF.Identity), r=['pb0'], w=['GTt'])
                for half in range(2):
                    P.op('pe', lambda e, half=half: e.matmul(pinit[half][:], GTt[:], BD[:, half * 512:(half + 1) * 512], start=True, stop=True),
                         r=['GTt'], w=[f'pb{1 + half}'])
                    hs = slice(half * 512, (half + 1) * 512)
                    P.op('dve', lambda e, half=half, hs=hs, acc=acc, ti=ti, k2=k2: e.scalar_tensor_tensor(out=acc[:, hs], in0=Y4[k2][0][:, hs], scalar=GK[:, ti, 0:1], in1=pinit[half][:],
                                                                                                       op0=ALU.mult, op1=ALU.add),
                         r=[f'pb{1 + half}', ('Y4', k2, 0)], w=[f'accf{k2}'])
                for k in range(1, 4):
                    P.op('dve', lambda e, k=k, acc=acc, ti=ti, k2=k2: e.scalar_tensor_tensor(out=acc[:], in0=Y4[k2][k][:], scalar=GK[:, ti, k:k + 1], in1=acc[:], op0=ALU.mult, op1=ALU.add),
                         r=[f'accf{k2}', ('Y4', k2, k)], w=[f'accf{k2}'])
                if debug:
                    P.dma('sp', lambda e, ti=ti, acc=acc: e.dma_start(out=dbg["dbg_acc"][:, ti * D:(ti + 1) * D], in_=acc[:]), r=[f'accf{k2}'])
                P.op('act', lambda e, acc=acc: e.activation(out=sqj[:], in_=acc[:], func=AF.Square, accum_out=ssq[:]), r=[f'accf{k2}'], w=['sqj', 'pn_ss'])
                P.op('act', lambda e: e.activation(out=rr[:], in_=ssq[:], func=AF.Ln, scale=1.0 / D, bias=EPS), r=['pn_ss'], w=['pn_rr'])
                P.op('act', lambda e: e.activation(out=rr[:], in_=rr[:], func=AF.Exp, scale=-0.5), r=['pn_rr'], w=['pn_rr'])
                P.op('dve', lambda e, acc=acc: e.scalar_tensor_tensor(out=acc[:], in0=acc[:], scalar=rr[:, 0:1], in1=GP2[:], op0=ALU.mult, op1=ALU.mult),
                     r=['pn_rr', f'accf{k2}'], w=[f'accf{k2}'])
                P.op('dve', lambda e, acc=acc, ht=ht: e.tensor_tensor(out=acc[:], in0=acc[:], in1=ht[:], op=ALU.add), r=[f'accf{k2}', f'ht{k2}'], w=[f'accf{k2}'])
                P.dma('sp', lambda e, ti=ti, acc=acc: e.dma_start(out=y_d[ti * 128:(ti + 1) * 128, :], in_=acc[:]), r=[f'accf{k2}', f'ht{k2}'], w=[('y', ti)])
            P.barrier()
            P.emit(block)


def build_mixer(nc, P, st, block, L, debug):
    sb = L['sb']; pbanks = L['pbanks']; ident = L['ident']; identb = L['identb']; ones_b = L['ones_b']
    S1, B1, GP1 = L['S1'], L['B1'], L['GP1']
    x_d, y_d = L['x_d'], L['y_d']
    dbg = L['dbg']; dbg_dump = L['dbg_dump']; prenorm_tile = L['prenorm_tile']
    win_d, walpha_d, balpha_d, glag_d = L['win_d'], L['walpha_d'], L['balpha_d'], L['glag_d']
    lre_d, lim_d, ldt_d = L['lre_d'], L['lim_d'], L['ldt_d']
    bre_d, bim_d, cre_d, cim_d, s5d_d = L['bre_d'], L['bim_d'], L['cre_d'], L['cim_d'], L['s5d_d']
    gluw_d, glub_d, wout_d = L['gluw_d'], L['glub_d'], L['wout_d']
    PB_ = ['pb%d' % i for i in range(8)]
    X = mybir.AxisListType.X
    issue_cast_pass = L['issue_cast_pass']

    with ExitStack() as pm:
        MIXIN = sb("MIXIN", [128, 8, T], BF16, stack=pm)
        UT = sb("UT", [128, 4, T], BF16, stack=pm)

        with ExitStack() as m1:
            HN = sb("HN", [128, 8, T], BF16, stack=m1)
            WIN = sb("WIN", [128, 8, 2064], BF16, stack=m1)
            ALR = sb("ALR", [16, T], BF16, stack=m1)
            WA = sb("WA", [16, 256], BF16, stack=m1)
            nba = sb("nba", [64, 4], stack=m1)
            glag = sb("glag", [128, 1], stack=m1)
            xts = [sb(f"xt{i}", [128, D], stack=m1) for i in range(2)]
            ssq2 = [sb(f"pn_ss{i}", [128, 1], stack=m1) for i in range(2)]
            rr2 = [sb(f"pn_rr{i}", [128, 1], stack=m1) for i in range(2)]
            xs2 = [sb(f"pn_xs{i}", [128, D], stack=m1) for i in range(2)]
            for kc in range(8):
                for (c0, c1) in ((0, 1032), (1032, 2064)):
                    P.dma('pool', lambda e, kc=kc, c0=c0, c1=c1: e.dma_start(out=WIN[:, kc, c0:c1], in_=win_d[kc * 128:(kc + 1) * 128, c0:c1]),
                          w=[('WIN', kc, int(c0 > 0))])
            P.dma('pool', lambda e: e.dma_start(out=WA[:], in_=walpha_d[:, :]), w=['WA'])
            P.dma('sp', lambda e: e.dma_start(out=nba[:], in_=balpha_d[:, :]), w=['nba'])
            P.dma('sp', lambda e: e.dma_start(out=glag[:], in_=glag_d[:, :]), w=['glag'])
            P.op('dve', lambda e: e.tensor_scalar(out=nba[:], in0=nba[:], scalar1=-1.0, scalar2=None, op0=ALU.mult), r=['nba'], w=['nba'])
            pst = [pbanks[0], pbanks[1]]
            for ti in range(NT):
                xt = xts[ti % 2]
                P.dma('sp', lambda e, ti=ti, xt=xt: e.dma_start(out=xt[:], in_=x_d[ti * 128:(ti + 1) * 128, :]), w=[f'xt{ti % 2}'])
                prenorm_tile(xt[:], [f'xt{ti % 2}'], ti, HN, lambda kc, ti_: ('HN', kc, ti_ // 4), S1, B1, (ssq2[ti % 2], rr2[ti % 2], xs2[ti % 2]), pst, sx=str(ti % 2))
            if debug:
                pass
            WINK = [('WIN', kc) for kc in range(8)]

            pj = [0]

            def proj(cols, M, tg_or_n, token_major=False):
                bi = pj[0] % 2
                pj[0] += 1
                bank = pbanks[bi]
                key = PB_[bi]
                return bank, key

            for tg in range(4):
                bank, key = proj(None, None, None)
                for kc in range(8):
                    P.op('pe', lambda e, kc=kc, tg=tg, bank=bank: e.matmul(bank[0:16, :], WIN[:, kc, 1536:1552], HN[:, kc, tg * 512:(tg + 1) * 512],
                                                                           start=(kc == 0), stop=(kc == 7)),
                         r=[('WIN', kc, 0), ('WIN', kc, 1), ('HN', kc, tg)], w=[key], sig=(kc == 7))
                P.op('act', lambda e, tg=tg, bank=bank: e.activation(out=ALR[:, tg * 512:(tg + 1) * 512], in_=bank[0:16, :], func=AF.Identity),
                     r=[key], w=[('ALR', tg)])
            for cc in range(4):
                for tg in range(4):
                    bank, key = proj(None, None, None)
                    for kc in range(8):
                        P.op('pe', lambda e, kc=kc, tg=tg, cc=cc, bank=bank: e.matmul(
                            bank[:], WIN[:, kc, 1552 + cc * 128:1552 + (cc + 1) * 128], HN[:, kc, tg * 512:(tg + 1) * 512], start=(kc == 0), stop=(kc == 7)),
                            r=[('WIN', kc, 0), ('WIN', kc, 1), ('HN', kc, tg)], w=[key], sig=(kc == 7))
                    P.op('act', lambda e, tg=tg, cc=cc, bank=bank: e.activation(out=UT[:, cc, tg * 512:(tg + 1) * 512], in_=bank[:], func=AF.Identity),
                         r=[key], w=[('UT', cc, tg)])

            with ExitStack() as g:
                T1 = sb("T1", [64, T], stack=g)
                T2 = sb("T2", [64, T], stack=g)
                QD = sb("QD", [64, T], BF16, stack=g)
                KI = sb("KI", [64, T], BF16, stack=g)
                KE = sb("KE", [64, T], BF16, stack=g)
                KEt = sb("KEt", [64, 32, 64], BF16, stack=g)
                V = sb("V", [64, 32, 128], BF16, stack=g)
                SOG = sb("SOG", [128, T], BF16, stack=g)
                Sf = [sb(f"Sf{i}", [64, 128], stack=g) for i in range(2)]
                Sb = sb("Sb", [64, 32, 128], BF16, stack=g)
                mask01 = sb("mask01", [64, T], BF16, stack=g)
                cmask = sb("cmask", [64, 64], stack=g)
                scTm = [sb(f"scTm{i}", [64, 8, 64], BF16, stack=g) for i in range(2)]
                sq = sb("sq", [128, 512], BF16, stack=g)
                rn = sb("rn", [128, 512], stack=g)
                tt_ = sb("tt_", [128, 512], stack=g)
                P.op('pool', lambda e: e.memset(mask01[:], 1.0), w=['mask01'])
                P.op('pool', lambda e: e.memset(mask01[:, 0::64], 0.0), w=['mask01'])
                P.op('pool', lambda e: e.memset(cmask[:], 1.0), w=['cmask'])
                P.op('pool', lambda e: e.affine_select(out=cmask[:], in_=cmask[:], pattern=[[1, 64]], compare_op=ALU.is_ge,
                                                       fill=0.0, base=0, channel_multiplier=-1), r=['cmask'], w=['cmask'])
                T1v = T1[:].rearrange("p (n t) -> p n t", t=64)
                issue_cast_pass(0, NE)
                for h in range(4):
                    for tg in range(4):
                        bank, key = proj(None, None, None)
                        P.op('pe', lambda e, tg=tg, h=h, bank=bank: e.matmul(bank[0:64, :], WA[:, h * 64:(h + 1) * 64], ALR[:, tg * 512:(tg + 1) * 512],
                                                                             start=True, stop=True),
                             r=['WA', ('ALR', tg)], w=[key])
                        P.op('act', lambda e, tg=tg, h=h, bank=bank: e.activation(out=T1[:, tg * 512:(tg + 1) * 512], in_=bank[0:64, :], func=AF.Exp,
                                                                                  scale=-1.0, bias=nba[:, h:h + 1]),
                             r=[key, 'nba'], w=['T1'])
                    P.op('act', lambda e: e.activation(out=T1[:], in_=T1[:], func=AF.Ln, bias=1.0, scale=1.0), r=['T1'], w=['T1'])
                    P.op('dve', lambda e: e.tensor_tensor_scan(out=T2[:], data0=mask01[:], data1=T1[:], initial=0.0, op0=ALU.mult, op1=ALU.add),
                         r=['T1', 'mask01'], w=['T2'])
                    P.op('act', lambda e: e.activation(out=T1[:], in_=T2[:], func=AF.Exp, scale=-1.0 / 16.0), r=['T2'], w=['T1'])
                    P.op('act', lambda e: e.activation(out=T2[:], in_=T2[:], func=AF.Exp, scale=1.0 / 16.0), r=['T2'], w=['T2'])
                    for tg in range(4):
                        bank, key = proj(None, None, None)
                        for kc in range(8):
                            P.op('pe', lambda e, kc=kc, tg=tg, h=h, bank=bank: e.matmul(bank[0:64, :], WIN[:, kc, h * 64:(h + 1) * 64], HN[:, kc, tg * 512:(tg + 1) * 512],
                                                                                        start=(kc == 0), stop=(kc == 7)),
                                 r=[('WIN', kc, 0), ('WIN', kc, 1), ('HN', kc, tg)], w=[key], sig=(kc == 7))
                        P.op('dve', lambda e, tg=tg, bank=bank: e.scalar_tensor_tensor(out=QD[:, tg * 512:(tg + 1) * 512], in0=bank[0:64, :], scalar=0.125,
                                                                                       in1=T1[:, tg * 512:(tg + 1) * 512], op0=ALU.mult, op1=ALU.mult),
                             r=[key, 'T1'], w=[('QD', tg)])
                    for tg in range(4):
                        bank, key = proj(None, None, None)
                        for kc in range(8):
                            P.op('pe', lambda e, kc=kc, tg=tg, h=h, bank=bank: e.matmul(bank[0:64, :], WIN[:, kc, 256 + h * 64:256 + (h + 1) * 64],
                                                                                        HN[:, kc, tg * 512:(tg + 1) * 512], start=(kc == 0), stop=(kc == 7)),
                                 r=[('WIN', kc, 0), ('WIN', kc, 1), ('HN', kc, tg)], w=[key], sig=(kc == 7))
                        P.op('dve', lambda e, tg=tg, bank=bank: e.tensor_tensor(out=KI[:, tg * 512:(tg + 1) * 512], in0=bank[0:64, :],
                                                                                in1=T2[:, tg * 512:(tg + 1) * 512], op=ALU.mult),
                             r=[key, 'T2'], w=[('KI', tg)])
                    P.op('dve', lambda e: e.tensor_tensor(out=KE[:].rearrange("p (n t) -> p n t", t=64), in0=KI[:].rearrange("p (n t) -> p n t", t=64),
                                                           in1=T1v[:, :, 63:64].to_broadcast([64, 32, 64]), op=ALU.mult),
                         r=[('KI', tg) for tg in range(4)] + ['T1'], w=['KE'])
                    ptb = pbanks[3][:].bitcast(BF16)
                    for n8 in range(4):
                        for q in range(8):
                            n = n8 * 8 + q
                            P.op('pe', lambda e, n=n, q=q: e.transpose(out=ptb[0:64, q * 64:(q + 1) * 64], in_=KE[:, n * 64:(n + 1) * 64], identity=identb[0:64, 0:64]),
                                 r=['KE', 'identb'], w=[PB_[3]], sig=(q == 7))
                        P.op('act', lambda e, n8=n8: e.activation(out=KEt[:, n8 * 8:(n8 + 1) * 8, :], in_=ptb[0:64, 0:512].rearrange("p (a b) -> p a b", b=64), func=AF.Identity),
                             r=[PB_[3]], w=[('KEt', n8)])
                    for n4 in range(8):
                        bank, key = proj(None, None, None)
                        for q in range(4):
                            n = n4 * 4 + q
                            for kc in range(8):
                                P.op('pe', lambda e, kc=kc, n=n, q=q, h=h, bank=bank: e.matmul(
                                    bank[0:64, q * 128:(q + 1) * 128], HN[:, kc, n * 64:(n + 1) * 64], WIN[:, kc, 512 + h * 128:512 + (h + 1) * 128],
                                    start=(kc == 0), stop=(kc == 7)),
                                    r=[('WIN', kc, 0), ('WIN', kc, 1), ('HN', kc, n // 8)], w=[key], sig=(kc == 7 and q == 3))
                        P.op('act', lambda e, n4=n4, bank=bank: e.activation(out=V[:, n4 * 4:(n4 + 1) * 4, :], in_=bank[0:64, :].rearrange("p (a b) -> p a b", b=128), func=AF.Identity),
                             r=[key], w=[('V', n4)])
                    for tg in range(4):
                        bank, key = proj(None, None, None)
                        for kc in range(8):
                            P.op('pe', lambda e, kc=kc, tg=tg, h=h, bank=bank: e.matmul(bank[:], WIN[:, kc, 1024 + h * 128:1024 + (h + 1) * 128],
                                                                                        HN[:, kc, tg * 512:(tg + 1) * 512], start=(kc == 0), stop=(kc == 7)),
                                 r=[('WIN', kc, 0), ('WIN', kc, 1), ('HN', kc, tg)], w=[key], sig=(kc == 7))
                        P.op('act', lambda e, tg=tg, bank=bank: e.activation(out=SOG[:, tg * 512:(tg + 1) * 512], in_=bank[:], func=AF.Silu), r=[key], w=[('SOG', tg)])
                    for n8 in range(4):
                        bi = 2 + (n8 % 2)
                        bank = pbanks[bi]
                        for q in range(8):
                            n = n8 * 8 + q
                            P.op('pe', lambda e, n=n, q=q, bank=bank: e.matmul(bank[0:64, q * 64:(q + 1) * 64], KI[:, n * 64:(n + 1) * 64], QD[:, n * 64:(n + 1) * 64],
                                                                               start=True, stop=True),
                                 r=[('KI', n // 8), ('QD', n // 8)], w=[PB_[bi]], sig=(q == 7))
                        P.op('dve', lambda e, n8=n8, bank=bank: e.tensor_tensor(out=scTm[n8 % 2][:], in0=bank[0:64, :].rearrange("p (a b) -> p a b", b=64),
                                                                                in1=cmask[:].unsqueeze(1).to_broadcast([64, 8, 64]), op=ALU.mult),
                             r=[PB_[bi], 'cmask'], w=[('scTm', n8 % 2)])
                        if n8 == 0:
                            P.op('dve', lambda e: e.memset(Sf[0][:], 0.0), w=['Sf0'])
                            P.op('dve', lambda e: e.memset(Sb[:, 0, :], 0.0), w=[('Sb', 0)])
                            for n4 in range(8):
                                dbi = 4 + (n4 % 2)
                                dbank = pbanks[dbi]
                                for q in range(4):
                                    n = n4 * 4 + q
                                    if n == 31:
                                        continue
                                    P.op('pe', lambda e, n=n, q=q, dbank=dbank: e.matmul(dbank[0:64, q * 128:(q + 1) * 128], KEt[:, n, :], V[:, n, :], start=True, stop=True),
                                         r=[('KEt', n // 8), ('V', n // 4)], w=[PB_[dbi]], sig=(q == 3 or n == 30))
                                for q in range(4):
                                    n = n4 * 4 + q
                                    if n == 31:
                                        continue
                                    so, sn = Sf[n % 2], Sf[(n + 1) % 2]
                                    P.op('dve', lambda e, n=n, q=q, so=so, sn=sn, dbank=dbank: e.scalar_tensor_tensor(
                                        out=sn[:], in0=so[:], scalar=T1v[:, n, 63:64], in1=dbank[0:64, q * 128:(q + 1) * 128], op0=ALU.mult, op1=ALU.add),
                                        r=[f'Sf{n % 2}', 'T1', PB_[dbi]], w=[f'Sf{(n + 1) % 2}'])
                                    P.op('act', lambda e, n=n, sn=sn: e.activation(out=Sb[:, n + 1, :], in_=sn[:], func=AF.Identity),
                                         r=[f'Sf{(n + 1) % 2}'], w=[('Sb', n + 1)])
                        tg = n8
                        po = pbanks[6]
                        for q in range(8):
                            n = n8 * 8 + q
                            P.op('pe', lambda e, n=n, q=q, n8=n8: e.matmul(po[:, q * 64:(q + 1) * 64], V[:, n, :], scTm[n8 % 2][:, q, :], start=True, stop=False),
                                 r=[('V', n // 4), ('scTm', n8 % 2)], w=[PB_[6]], sig=False)
                            P.op('pe', lambda e, n=n, q=q: e.matmul(po[:, q * 64:(q + 1) * 64], Sb[:, n, :], QD[:, n * 64:(n + 1) * 64], start=False, stop=True),
                                 r=[('Sb', n), ('QD', n // 8)], w=[PB_[6]], sig=(q == 7))
                        P.op('act', lambda e: e.activation(out=sq[:], in_=po[:], func=AF.Square), r=[PB_[6]], w=['sq'])
                        pss = pbanks[7]
                        P.op('pe', lambda e: e.matmul(pss[:], ones_b[:], sq[:], start=True, stop=True), r=['sq', 'ones_b'], w=[PB_[7]])
                        P.op('act', lambda e: e.activation(out=rn[:], in_=pss[:], func=AF.Ln, scale=1.0 / 128.0, bias=EPS), r=[PB_[7]], w=['rn'])
                        P.op('act', lambda e: e.activation(out=rn[:], in_=rn[:], func=AF.Exp, scale=-0.5), r=['rn'], w=['rn'])
                        P.op('dve', lambda e: e.scalar_tensor_tensor(out=tt_[:], in0=po[:], scalar=glag[:, 0:1], in1=rn[:], op0=ALU.mult, op1=ALU.mult),
                             r=[PB_[6], 'rn', 'glag'], w=['tt_'])
                        P.op('dve', lambda e, tg=tg, h=h: e.tensor_tensor(out=MIXIN[:, h, tg * 512:(tg + 1) * 512], in0=tt_[:], in1=SOG[:, tg * 512:(tg + 1) * 512], op=ALU.mult),
                             r=['tt_', ('SOG', tg)], w=[('MIXIN', h, tg)])
            P.barrier()
            P.emit(block)

        with ExitStack() as m2:
            def t16(name):
                return sb(name, [128, 16], stack=m2)
            lre = t16("lre"); lim = t16("lim"); dtt = t16("dtt"); mag = t16("mag")
            A2 = sb("A2", [128, 32], stack=m2); K2 = sb("K2", [128, 32], stack=m2); KI2 = sb("KI2", [128, 32], I32, stack=m2)
            M2_ = sb("M2_", [128, 32], stack=m2); SN = sb("SN", [128, 32], stack=m2)
            den = t16("den"); am1 = t16("am1"); fre = t16("fre"); fim = t16("fim"); tA = t16("tA"); tB = t16("tB")
            PW = sb("PW", [128, 11, 3, 16], stack=m2)
            bre = sb("bre", [128, 16, 16], stack=m2); bim = sb("bim", [128, 16, 16], stack=m2)
            cre = sb("cre", [128, 16, 16], stack=m2); cim = sb("cim", [128, 16, 16], stack=m2)
            bbr = sb("bbr", [128, 16, 16], stack=m2); bbi = sb("bbi", [128, 16, 16], stack=m2); tb3 = sb("tb3", [128, 16, 16], stack=m2)
            s5d = sb("s5d", [128, 4], stack=m2); glub = sb("glub", [128, 4], stack=m2)
            PBm = sb("PBm", [128, 16, 2, 128], stack=m2)
            WB = sb("WB", [128, 32, 128], BF16, stack=m2)
            WC = sb("WC", [128, 16, 2, 128], BF16, stack=m2)
            GW = sb("GW", [128, 4, 512], BF16, stack=m2)
            GWf = sb("GWf", [128, 512], stack=m2)
            XP = [sb(f"XP{i}", [128, 2, T], stack=m2) for i in range(2)]
            XS = [[XP[i][:, 0, :], XP[i][:, 1, :]] for i in range(2)]
            GT_ = sb("GT_", [128, T], stack=m2)
            XBF = sb("XBF", [128, 4, 2, T], BF16, stack=m2)
            YT = sb("YT", [128, T], stack=m2)
            YG = sb("YG", [128, 4, T], BF16, stack=m2)
            sgl = sb("sgl", [128, 512], stack=m2)
            for nm, t_, d_ in (("lre", lre, lre_d), ("lim", lim, lim_d), ("dtt", dtt, ldt_d), ("s5d", s5d, s5d_d), ("glub", glub, glub_d)):
                P.dma('sp', lambda e, t_=t_, d_=d_: e.dma_start(out=t_[:], in_=d_[:, :]), w=[nm])
            for nm, t_, d_ in (("bre", bre, bre_d), ("bim", bim, bim_d), ("cre", cre, cre_d), ("cim", cim, cim_d)):
                P.dma('sp', lambda e, t_=t_, d_=d_: e.dma_start(out=t_[:], in_=d_[:, :, :]), w=[nm])
            for cc in range(4):
                P.dma('sp', lambda e, cc=cc: e.dma_start(out=GWf[:], in_=gluw_d[cc * 128:(cc + 1) * 128, :]), w=['GWf'])
                P.op('act', lambda e, cc=cc: e.activation(out=GW[:, cc, :], in_=GWf[:], func=AF.Identity), r=['GWf'], w=[('GW', cc)])

            def dv(fn, r, w):
                return P.op('dve', fn, r=r, w=w)
            TWO_PI = 2.0 * math.pi
            P.op('act', lambda e: e.activation(out=dtt[:], in_=dtt[:], func=AF.Exp), r=['dtt'], w=['dtt'])
            dv(lambda e: e.tensor_tensor(out=tA[:], in0=lre[:], in1=dtt[:], op=ALU.mult), ['lre', 'dtt'], ['tA'])
            P.op('act', lambda e: e.activation(out=mag[:], in_=tA[:], func=AF.Exp), r=['tA'], w=['mag'])
            dv(lambda e: e.tensor_tensor(out=A2[:, 0:16], in0=lim[:], in1=dtt[:], op=ALU.mult), ['lim', 'dtt'], ['A2'])
            dv(lambda e: e.tensor_scalar(out=A2[:, 16:32], in0=A2[:, 0:16], scalar1=math.pi / 2.0, scalar2=None, op0=ALU.add), ['A2'], ['A2'])
            dv(lambda e: e.tensor_scalar(out=K2[:], in0=A2[:], scalar1=1.0 / TWO_PI, scalar2=None, op0=ALU.mult), ['A2'], ['K2'])
            dv(lambda e: e.tensor_copy(out=KI2[:], in_=K2[:]), ['K2'], ['KI2'])
            dv(lambda e: e.tensor_copy(out=K2[:], in_=KI2[:]), ['KI2'], ['K2'])
            dv(lambda e: e.scalar_tensor_tensor(out=A2[:], in0=K2[:], scalar=-TWO_PI, in1=A2[:], op0=ALU.mult, op1=ALU.add), ['K2', 'A2'], ['A2'])
            dv(lambda e: e.tensor_scalar(out=M2_[:], in0=A2[:], scalar1=math.pi, scalar2=None, op0=ALU.is_gt), ['A2'], ['M2_'])
            dv(lambda e: e.scalar_tensor_tensor(out=A2[:], in0=M2_[:], scalar=-TWO_PI, in1=A2[:], op0=ALU.mult, op1=ALU.add), ['M2_', 'A2'], ['A2'])
            dv(lambda e: e.tensor_scalar(out=M2_[:], in0=A2[:], scalar1=-math.pi, scalar2=None, op0=ALU.is_lt), ['A2'], ['M2_'])
            dv(lambda e: e.scalar_tensor_tensor(out=A2[:], in0=M2_[:], scalar=TWO_PI, in1=A2[:], op0=ALU.mult, op1=ALU.add), ['M2_', 'A2'], ['A2'])
            dv(lambda e: e.tensor_scalar(out=A2[:], in0=A2[:], scalar1=math.pi, scalar2=-math.pi, op0=ALU.min, op1=ALU.max), ['A2'], ['A2'])
            P.op('act', lambda e: e.activation(out=SN[:], in_=A2[:], func=AF.Sin), r=['A2'], w=['SN'])
            AR0 = PW[:, 0, 0, :]; AI0 = PW[:, 0, 1, :]; NAI0 = PW[:, 0, 2, :]
            dv(lambda e: e.tensor_tensor(out=AR0, in0=mag[:], in1=SN[:, 16:32], op=ALU.mult), ['mag', 'SN'], ['PW'])
            dv(lambda e: e.tensor_tensor(out=AI0, in0=mag[:], in1=SN[:, 0:16], op=ALU.mult), ['mag', 'SN'], ['PW'])
            dv(lambda e: e.tensor_scalar(out=NAI0, in0=AI0, scalar1=-1.0, scalar2=None, op0=ALU.mult), ['PW'], ['PW'])
            for k in range(1, 11):
                a_r = PW[:, k - 1, 0, :]; a_i = PW[:, k - 1, 1, :]
                n_r = PW[:, k, 0, :]; n_i = PW[:, k, 1, :]; n_ni = PW[:, k, 2, :]
                dv(lambda e, a_r=a_r: e.tensor_tensor(out=tA[:], in0=a_r, in1=a_r, op=ALU.mult), ['PW'], ['tA'])
                dv(lambda e, a_i=a_i: e.tensor_tensor(out=tB[:], in0=a_i, in1=a_i, op=ALU.mult), ['PW'], ['tB'])
                dv(lambda e, n_r=n_r: e.tensor_tensor(out=n_r, in0=tA[:], in1=tB[:], op=ALU.subtract), ['tA', 'tB'], ['PW'])
                dv(lambda e, a_r=a_r, a_i=a_i: e.tensor_tensor(out=tA[:], in0=a_r, in1=a_i, op=ALU.mult), ['PW'], ['tA'])
                dv(lambda e, n_i=n_i: e.tensor_scalar(out=n_i, in0=tA[:], scalar1=2.0, scalar2=None, op0=ALU.mult), ['tA'], ['PW'])
                dv(lambda e, n_ni=n_ni: e.tensor_scalar(out=n_ni, in0=tA[:], scalar1=-2.0, scalar2=None, op0=ALU.mult), ['tA'], ['PW'])
            dv(lambda e: e.tensor_tensor(out=den[:], in0=lre[:], in1=lre[:], op=ALU.mult), ['lre'], ['den'])
            dv(lambda e: e.tensor_tensor(out=tA[:], in0=lim[:], in1=lim[:], op=ALU.mult), ['lim'], ['tA'])
            dv(lambda e: e.tensor_tensor(out=den[:], in0=den[:], in1=tA[:], op=ALU.add), ['den', 'tA'], ['den'])
            dv(lambda e: e.reciprocal(out=den[:], in_=den[:]), ['den'], ['den'])
            dv(lambda e: e.tensor_scalar(out=am1[:], in0=AR0, scalar1=-1.0, scalar2=None, op0=ALU.add), ['PW'], ['am1'])
            dv(lambda e: e.tensor_tensor(out=tA[:], in0=am1[:], in1=lre[:], op=ALU.mult), ['am1', 'lre'], ['tA'])
            dv(lambda e: e.tensor_tensor(out=tB[:], in0=AI0, in1=lim[:], op=ALU.mult), ['PW', 'lim'], ['tB'])
            dv(lambda e: e.tensor_tensor(out=tA[:], in0=tA[:], in1=tB[:], op=ALU.add), ['tA', 'tB'], ['tA'])
            dv(lambda e: e.tensor_tensor(out=fre[:], in0=tA[:], in1=den[:], op=ALU.mult), ['tA', 'den'], ['fre'])
            dv(lambda e: e.tensor_tensor(out=tA[:], in0=AI0, in1=lre[:], op=ALU.mult), ['PW', 'lre'], ['tA'])
            dv(lambda e: e.tensor_tensor(out=tB[:], in0=am1[:], in1=lim[:], op=ALU.mult), ['am1', 'lim'], ['tB'])
            dv(lambda e: e.tensor_tensor(out=tA[:], in0=tA[:], in1=tB[:], op=ALU.subtract), ['tA', 'tB'], ['tA'])
            dv(lambda e: e.tensor_tensor(out=fim[:], in0=tA[:], in1=den[:], op=ALU.mult), ['tA', 'den'], ['fim'])
            freb = fre[:].unsqueeze(2).to_broadcast([128, 16, 16]); fimb = fim[:].unsqueeze(2).to_broadcast([128, 16, 16])
            dv(lambda e: e.tensor_tensor(out=bbr[:], in0=bre[:], in1=freb, op=ALU.mult), ['bre', 'fre'], ['bbr'])
            dv(lambda e: e.tensor_tensor(out=tb3[:], in0=bim[:], in1=fimb, op=ALU.mult), ['bim', 'fim'], ['tb3'])
            dv(lambda e: e.tensor_tensor(out=bbr[:], in0=bbr[:], in1=tb3[:], op=ALU.subtract), ['bbr', 'tb3'], ['bbr'])
            dv(lambda e: e.tensor_tensor(out=bbi[:], in0=bim[:], in1=freb, op=ALU.mult), ['bim', 'fre'], ['bbi'])
            dv(lambda e: e.tensor_tensor(out=tb3[:], in0=bre[:], in1=fimb, op=ALU.mult), ['bre', 'fim'], ['tb3'])
            dv(lambda e: e.tensor_tensor(out=bbi[:], in0=bbi[:], in1=tb3[:], op=ALU.add), ['bbi', 'tb3'], ['bbi'])
            P.op('dve', lambda e: e.memset(PBm[:], 0.0), w=['PBm'])
            P.op('dve', lambda e: e.memset(WC[:], 0.0), w=['WC'])
            for j in range(4):
                for ri, (bsrc, bkey, csrc, ckey, csign) in enumerate(((bbr, 'bbr', cre, 'cre', 1.0), (bbi, 'bbi', cim, 'cim', -1.0))):
                    for hf in range(2):
                        prt = slice(hf * 64, (hf + 1) * 64)
                        cs = slice(32 * j + 16 * hf, 32 * j + 16 * hf + 16)
                        dv(lambda e, bsrc=bsrc, prt=prt, cs=cs, ri=ri, j=j: e.tensor_copy(out=PBm[prt, j::4, ri, cs], in_=bsrc[prt, j::4, :]), [bkey, 'PBm'], ['PBm'])
                        dv(lambda e, csrc=csrc, prt=prt, cs=cs, ri=ri, j=j, csign=csign: e.tensor_scalar(
                            out=WC[prt, j::4, ri, cs], in0=csrc[prt, j::4, :], scalar1=csign, scalar2=None, op0=ALU.mult), [ckey, 'WC'], ['WC'])
            PBf = PBm[:].rearrange("p m r c -> p (m r) c")
            for i4 in range(8):
                bi = i4 % 2
                bank = pbanks[bi]
                for q in range(4):
                    idx = i4 * 4 + q
                    P.op('pe', lambda e, idx=idx, q=q, bank=bank: e.transpose(out=bank[:, q * 128:(q + 1) * 128], in_=PBf[:, idx, :], identity=ident[:]),
                         r=['PBm', 'ident'], w=[PB_[bi]], sig=(q == 3))
                P.op('act', lambda e, i4=i4, bank=bank: e.activation(out=WB[:, i4 * 4:(i4 + 1) * 4, :], in_=bank[:].rearrange("p (a b) -> p a b", b=128), func=AF.Identity),
                     r=[PB_[bi]], w=['WB'])
            WCf = WC[:].rearrange("p m r c -> p (m r) c")

            pjc = [0]
            for cc in range(4):
                for jp in range(2):
                    ms = [4 * cc + 2 * jp, 4 * cc + 2 * jp + 1]
                    for m in ms:
                        for ri in range(2):
                            for tg in range(4):
                                bi = pjc[0] % 2
                                pjc[0] += 1
                                bank = pbanks[bi]
                                P.op('pe', lambda e, m=m, ri=ri, tg=tg, cc=cc, bank=bank: e.matmul(bank[:], WB[:, 2 * m + ri, :], UT[:, cc, tg * 512:(tg + 1) * 512], start=True, stop=True),
                                     r=['WB'], w=[PB_[bi]])
                                P.op('act', lambda e, ri=ri, tg=tg, bank=bank, m=m: e.activation(out=XP[m % 2][:, ri, tg * 512:(tg + 1) * 512], in_=bank[:], func=AF.Identity),
                                     r=[PB_[bi]], w=[f'X{m % 2}' + 'ri'[ri]])

                    def bk_level(d, start):
                        step = 1 << (d + 1)
                        hop = 1 << d
                        if start >= T:
                            return
                        cnt = (T - 1 - start) // step + 1
                        w_ = lambda Xt: Xt[:, start:start + (cnt - 1) * step + 1:step]
                        r_ = lambda Xt: Xt[:, start - hop:start - hop + (cnt - 1) * step + 1:step]
                        w2_ = lambda Xp: Xp[:, :, start:start + (cnt - 1) * step + 1:step]
                        r2_ = lambda Xp: Xp[:, :, start - hop:start - hop + (cnt - 1) * step + 1:step]
                        for phase in range(2):
                            for m in ms:
                                Xp = XP[m % 2]
                                Xr, Xi = XS[m % 2]
                                kr, ki = f'X{m % 2}r', f'X{m % 2}i'
                                ARk = PW[:, d, 0, m:m + 1]; AIk = PW[:, d, 1, m:m + 1]; NAIk = PW[:, d, 2, m:m + 1]
                                if phase == 0:
                                    dv(lambda e, Xp=Xp, ARk=ARk: e.scalar_tensor_tensor(out=w2_(Xp), in0=r2_(Xp), scalar=ARk, in1=w2_(Xp), op0=ALU.mult, op1=ALU.add), [kr, ki], [kr, ki])
                                else:
                                    dv(lambda e, Xr=Xr, Xi=Xi, NAIk=NAIk: e.scalar_tensor_tensor(out=w_(Xr), in0=r_(Xi), scalar=NAIk, in1=w_(Xr), op0=ALU.mult, op1=ALU.add), [kr, ki], [kr])
                                    dv(lambda e, Xr=Xr, Xi=Xi, AIk=AIk: e.scalar_tensor_tensor(out=w_(Xi), in0=r_(Xr), scalar=AIk, in1=w_(Xi), op0=ALU.mult, op1=ALU.add), [kr, ki], [ki])
                    for d in range(11):
                        bk_level(d, (1 << (d + 1)) - 1)
                    for d in range(9, -1, -1):
                        bk_level(d, 3 * (1 << d) - 1)
                    for m in ms:
                        jm = m - 4 * cc
                        for ri in range(2):
                            P.op('act', lambda e, ri=ri, jm=jm, Xt=XS[m % 2][ri]: e.activation(out=XBF[:, jm, ri, :], in_=Xt, func=AF.Identity),
                                 r=[f'X{m % 2}' + 'ri'[ri]], w=[('XBF', jm, ri)])
                for tg in range(4):
                    bi = 2 + (tg % 2)
                    bank = pbanks[bi]
                    i_ = 0
                    for jm in range(4):
                        m = 4 * cc + jm
                        for ri in range(2):
                            P.op('pe', lambda e, m=m, ri=ri, jm=jm, tg=tg, bank=bank, i_=i_: e.matmul(bank[:], WCf[:, 2 * m + ri, :], XBF[:, jm, ri, tg * 512:(tg + 1) * 512],
                                                                                                      start=(i_ == 0), stop=(i_ == 7)),
                                 r=['WC', ('XBF', jm, ri)], w=[PB_[bi]], sig=(i_ == 7))
                            i_ += 1
                    dv(lambda e, tg=tg, cc=cc, bank=bank: e.scalar_tensor_tensor(out=YT[:, tg * 512:(tg + 1) * 512], in0=UT[:, cc, tg * 512:(tg + 1) * 512], scalar=s5d[:, cc:cc + 1],
                                                                                 in1=bank[:], op0=ALU.mult, op1=ALU.add),
                       [PB_[bi], 's5d'], [('YT', tg)])
                YTK = [('YT', tg) for tg in range(4)]
                P.op('act', lambda e: e.activation(out=GT_[:], in_=YT[:], func=AF.Square), r=YTK, w=['GT_'])
                P.op('act', lambda e: e.activation(out=GT_[:], in_=GT_[:], func=AF.Identity, scale=0.044715, bias=1.0), r=['GT_'], w=['GT_'])
                P.op('dve', lambda e: e.tensor_tensor(out=GT_[:], in0=GT_[:], in1=YT[:], op=ALU.mult), r=['GT_'] + YTK, w=['GT_'])
                P.op('act', lambda e: e.activation(out=GT_[:], in_=GT_[:], func=AF.Sigmoid, scale=2.0 * math.sqrt(2.0 / math.pi)), r=['GT_'], w=['GT_'])
                P.op('dve', lambda e, cc=cc: e.tensor_tensor(out=YG[:, cc, :], in0=GT_[:], in1=YT[:], op=ALU.mult), r=['GT_'] + YTK, w=[('YG', cc)])
            for oc in range(4):
                for tg in range(4):
                    bi = 4 + (tg % 2)
                    bank = pbanks[bi]
                    for cc in range(4):
                        P.op('pe', lambda e, oc=oc, tg=tg, cc=cc, bank=bank: e.matmul(bank[:], GW[:, cc, oc * 128:(oc + 1) * 128], YG[:, cc, tg * 512:(tg + 1) * 512],
                                                                                      start=(cc == 0), stop=(cc == 3)),
                             r=[('GW', cc), ('YG', cc)], w=[PB_[bi]], sig=(cc == 3))
                    P.op('act', lambda e, oc=oc, bank=bank: e.activation(out=sgl[:], in_=bank[:], func=AF.Sigmoid, bias=glub[:, oc:oc + 1], scale=1.0),
                         r=[PB_[bi], 'glub'], w=['sgl'])
                    dv(lambda e, oc=oc, tg=tg: e.tensor_tensor(out=MIXIN[:, 4 + oc, tg * 512:(tg + 1) * 512], in0=YG[:, oc, tg * 512:(tg + 1) * 512], in1=sgl[:], op=ALU.mult),
                       ['sgl', ('YG', oc)], [('MIXIN', 4 + oc, tg)])
            P.barrier()
            P.emit(block)

        if debug:
            for c in range(8):
                pass

        with ExitStack() as m3:
            WO = sb("WO", [128, 8, D], BF16, stack=m3)
            WOf = [sb(f"WOf{i}", [128, D], stack=m3) for i in range(2)]
            mixt = [sb(f"mixt{i}", [128, D], stack=m3) for i in range(4)]
            xts = [sb(f"xt{i}", [128, D], stack=m3) for i in range(NT)]
            jk = sb("jk", [128, D], stack=m3)
            ssq = sb("pn_ss", [128, 1], stack=m3)
            rr = sb("pn_rr", [128, 1], stack=m3)
            for c in range(8):
                P.dma('sp', lambda e, c=c: e.dma_start(out=WOf[c % 2][:], in_=wout_d[c * 128:(c + 1) * 128, :]), w=[f'WOf{c % 2}'])
                P.op('act', lambda e, c=c: e.activation(out=WO[:, c, :], in_=WOf[c % 2][:], func=AF.Identity), r=[f'WOf{c % 2}'], w=[('WO', c)])
            if debug:
                dmx = sb("dmx", [128, T], stack=m3)
                for c in range(8):
                    P.op('dve', lambda e, c=c: e.tensor_copy(out=dmx[:], in_=MIXIN[:, c, :]), w=['dmx'])
                    P.dma('sp', lambda e, c=c: e.dma_start(out=dbg["dbg_mixin"][:, c * T:(c + 1) * T], in_=dmx[:]), r=['dmx'])
            for ti in range(NT):
                P.dma('act', lambda e, ti=ti: e.dma_start(out=xts[ti][:], in_=x_d[ti * 128:(ti + 1) * 128, :]), w=[f'xtm{ti}'])
            for ti in range(NT):
                k = ti % 4
                mt = mixt[k]; xt = xts[ti]
                for half in range(2):
                    bi = (ti * 2 + half) % 4
                    bank = pbanks[bi]
                    for c in range(8):
                        P.op('pe', lambda e, c=c, ti=ti, half=half, bank=bank: e.matmul(bank[:], MIXIN[:, c, ti * 128:(ti + 1) * 128], WO[:, c, half * 512:(half + 1) * 512],
                                                                                        start=(c == 0), stop=(c == 7)),
                             r=[('WO', c)], w=[PB_[bi]], sig=(c == 7))
                    P.op('act', lambda e, mt=mt, half=half, bank=bank: e.activation(out=mt[:, half * 512:(half + 1) * 512], in_=bank[:], func=AF.Identity),
                         r=[PB_[bi]], w=[f'mixt{k}'])
                P.op('act', lambda e, mt=mt: e.activation(out=jk[:], in_=mt[:], func=AF.Square, accum_out=ssq[:]), r=[f'mixt{k}'], w=['jk', 'pn_ss'])
                P.op('act', lambda e: e.activation(out=rr[:], in_=ssq[:], func=AF.Ln, scale=1.0 / D, bias=EPS), r=['pn_ss'], w=['pn_rr'])
                P.op('act', lambda e: e.activation(out=rr[:], in_=rr[:], func=AF.Exp, scale=-0.5), r=['pn_rr'], w=['pn_rr'])
                P.op('dve', lambda e, mt=mt: e.scalar_tensor_tensor(out=mt[:], in0=mt[:], scalar=rr[:, 0:1], in1=GP1[:], op0=ALU.mult, op1=ALU.mult),
                     r=[f'mixt{k}', 'pn_rr'], w=[f'mixt{k}'])
                P.op('dve', lambda e, mt=mt, xt=xt: e.tensor_tensor(out=mt[:], in0=mt[:], in1=xt[:], op=ALU.add), r=[f'mixt{k}', f'xtm{ti}'], w=[f'mixt{k}'])
                P.dma('sp', lambda e, ti=ti, mt=mt: e.dma_start(out=y_d[ti * 128:(ti + 1) * 128, :], in_=mt[:]), r=[f'mixt{k}'], w=[('y', ti)])
                if debug:
                    P.dma('sp', lambda e, ti=ti, mt=mt: e.dma_start(out=dbg["dbg_h"][ti * 128:(ti + 1) * 128, :], in_=mt[:]), r=[f'mixt{k}'])
            P.barrier()
            P.emit(block)


_NC_CACHE = {}


def make_in_maps(inp):
    f = lambda a: np.ascontiguousarray(np.asarray(a, dtype=np.float32))
    l = 0
    shared = {
        "ada_w": f(inp["ada_w"][l]),
        "ada_b_col": f(inp["ada_b"][l].reshape(48, 128).T),
        "ada_b_row": f(inp["ada_b"][l].reshape(1, 6 * D)),
        "mix_pre_g": f(inp["mix_pre_g"][l].reshape(8, 128).T),
        "ffn_pre_g": f(inp["ffn_pre_g"][l].reshape(8, 128).T),
        "mix_post_g": f(inp["mix_post_g"][l].reshape(1, D)),
        "ffn_post_g": f(inp["ffn_post_g"][l].reshape(1, D)),
        "w_in": f(inp["w_in"][l]),
        "w_alpha": f(inp["w_alpha"][l]),
        "b_alpha": f(inp["b_alpha"][l].reshape(4, 64).T),
        "gla_norm_g": f(inp["gla_norm_g"][l].reshape(128, 1)),
        "s5_lre": f(inp["s5_lambda_re"][l].reshape(16, 128).T),
        "s5_lim": f(inp["s5_lambda_im"][l].reshape(16, 128).T),
        "s5_ldt": f(np.repeat(np.asarray(inp["s5_log_dt"][l]), 64).reshape(16, 128).T),
        "s5_bre": f(np.asarray(inp["s5_b_re"][l]).reshape(16, 128, 16).transpose(1, 0, 2)),
        "s5_bim": f(np.asarray(inp["s5_b_im"][l]).reshape(16, 128, 16).transpose(1, 0, 2)),
        "s5_cre": f(np.asarray(inp["s5_c_re"][l]).transpose(0, 2, 1).reshape(16, 128, 16).transpose(1, 0, 2)),
        "s5_cim": f(np.asarray(inp["s5_c_im"][l]).transpose(0, 2, 1).reshape(16, 128, 16).transpose(1, 0, 2)),
        "s5_d": f(np.asarray(inp["s5_d"][l]).reshape(4, 128).T),
        "s5_glu_w": f(inp["s5_glu_w"][l]),
        "s5_glu_b": f(np.asarray(inp["s5_glu_b"][l]).reshape(4, 128).T),
        "w_out": f(inp["w_out"][l]),
        "router_w": f(inp["router_w"][l]),
        "router_b": f(np.asarray(inp["router_b"][l]).reshape(1, NE)),
        "exp_w_gu": f(inp["exp_w_gu"][l]),
        "exp_b_gu": f(np.asarray(inp["exp_b_gu"][l]).reshape(NE, 16, 128).transpose(2, 0, 1)),
        "exp_w_down": f(inp["exp_w_down"][l]),
        "exp_b_down": f(inp["exp_b_down"][l]),
    }
    x = np.asarray(inp["x"], dtype=np.float32)
    c = np.asarray(inp["c"], dtype=np.float32)
    maps = []
    for b in range(x.shape[0]):
        m = dict(shared)
        m["x"] = np.ascontiguousarray(x[b])
        m["c128"] = np.ascontiguousarray(c[b].reshape(8, 128).T)
        maps.append(m)
    return maps


def kernel(**inputs):
    maps = make_in_maps(inputs)
    if "nc" not in _NC_CACHE:
        _NC_CACHE["nc"] = build()
    nc = _NC_CACHE["nc"]
    res = run_bass_kernel_spmd(nc, maps, core_ids=list(range(len(maps))))
    out = np.stack([np.asarray(r["y"], dtype=np.float32) for r in res.results], axis=0)
    return out
```
